# Optimizing a Trainium2 kernel written in Bass

```python
import jax, jax.numpy as jnp
from jax import lax
import numpy as np

D_MODEL = 1024
BATCH = 8
SEQ = 4096
DEPTH = 1

CHUNK = 64
D_MIX = D_MODEL
D_A = D_MIX // 2
D_B = D_MIX - D_A
SGU_BLOCK = 128
SGU_GROUPS = 4
SGU_DIM = D_A // SGU_GROUPS
N_HEADS_B = 4
HEAD_DIM_B = D_B // N_HEADS_B
IDX_HEADS = 8
IDX_DIM = 64
MAX_TOPK = 256
QBLOCK = 128
NUM_BUCKETS = 32
MAX_DISTANCE = 128
D_FF = ((8 * D_MODEL // 3 + 255) // 256) * 256
EPS = 1e-6
SPLITS = (D_A, D_A, D_B, D_B, D_B, IDX_HEADS * IDX_DIM, IDX_DIM, IDX_HEADS)
D_IN = sum(SPLITS)

kernel_name = "hybrid_sgu_dsa_sandwich_block"


def _rmsnorm(x, g):
    xf = x.astype(jnp.float32)
    y = xf * lax.rsqrt(jnp.mean(xf * xf, axis=-1, keepdims=True) + EPS)
    return (y * g.astype(jnp.float32)).astype(x.dtype)


def _layernorm(x, g, b):
    xf = x.astype(jnp.float32)
    mu = jnp.mean(xf, axis=-1, keepdims=True)
    var = jnp.mean(jnp.square(xf - mu), axis=-1, keepdims=True)
    y = (xf - mu) * lax.rsqrt(var + EPS)
    return (y * g.astype(jnp.float32) + b.astype(jnp.float32)).astype(x.dtype)


def _t5_bucket(rel):
    nb = NUM_BUCKETS // 2
    ret = (rel > 0).astype(jnp.int32) * nb
    n = jnp.abs(rel)
    max_exact = nb // 2
    nf = jnp.maximum(n, 1).astype(jnp.float32)
    large = max_exact + (jnp.log(nf / max_exact) / np.float32(np.log(MAX_DISTANCE / max_exact))
                         * (nb - max_exact)).astype(jnp.int32)
    large = jnp.minimum(large, nb - 1)
    return ret + jnp.where(n < max_exact, n, large)


def _spatial_gating(u, v, ln_g, ln_b, w_s, b_s):
    B, S, _ = v.shape
    vn = _layernorm(v, ln_g, ln_b)
    vb = vn.reshape(B, S // SGU_BLOCK, SGU_BLOCK, SGU_GROUPS, SGU_DIM)
    pos_chunk = jnp.arange(SGU_BLOCK) // CHUNK
    mask = pos_chunk[None, :] <= pos_chunk[:, None]
    w = jnp.where(mask[None], w_s, jnp.zeros_like(w_s))
    mixed = jnp.einsum('gij,bcjgd->bcigd', w, vb) + jnp.transpose(b_s)[None, None, :, :, None]
    return u * mixed.reshape(B, S, D_A)


def _dsa_attention(q, k, v, iq, ik, iw, rel_bias):
    B, S, H, Dh = q.shape
    top_k = min(MAX_TOPK, S // 4)
    nb = S // QBLOCK
    s_chunk = jnp.arange(S) // CHUNK
    ikf = ik.astype(jnp.float32)

    def to_blocks(a):
        return jnp.moveaxis(a.reshape((B, nb, QBLOCK) + a.shape[2:]), 1, 0)

    def one_block(args):
        qb, iqb, iwb, blk = args
        t = blk * QBLOCK + jnp.arange(QBLOCK)
        t_chunk = t // CHUNK
        dots = jax.nn.relu(jnp.einsum('bqhd,bsd->bqhs', iqb.astype(jnp.float32), ikf)
                           * np.float32(IDX_DIM ** -0.5))
        score = jnp.einsum('bqh,bqhs->bqs', iwb.astype(jnp.float32) * np.float32(IDX_HEADS ** -0.5), dots)
        visible = s_chunk[None, :] <= t_chunk[:, None]
        score = jnp.where(visible[None], score, -jnp.inf)
        _, idx = lax.top_k(score, top_k)
        valid = (idx // CHUNK) <= t_chunk[None, :, None]
        k_sel = jax.vmap(lambda kb, ib: kb[ib])(k, idx)
        v_sel = jax.vmap(lambda vb, ib: vb[ib])(v, idx)
        bias = jnp.take(rel_bias, _t5_bucket(idx - t[None, :, None]), axis=0)
        logits = (jnp.einsum('bqhd,bqkhd->bqhk', qb, k_sel).astype(jnp.float32) * np.float32(Dh ** -0.5)
                  + jnp.moveaxis(bias, -1, 2).astype(jnp.float32))
        logits = jnp.where(valid[:, :, None, :], logits, -jnp.inf)
        p = jax.nn.softmax(logits, axis=-1).astype(v.dtype)
        return jnp.einsum('bqhk,bqkhd->bqhd', p, v_sel)

    out = lax.map(one_block, (to_blocks(q), to_blocks(iq), to_blocks(iw), jnp.arange(nb)))
    return jnp.moveaxis(out, 0, 1).reshape(B, S, H * Dh)


def setup_inputs(seed: int = 0) -> dict:
    key = jax.random.key(seed)
    ks = jax.random.split(key, 16)
    f32 = jnp.float32

    def gain(k, shape):
        return jnp.ones(shape, f32) + 0.01 * jax.random.normal(k, shape, f32)

    return {
        "x": jax.random.normal(ks[0], (BATCH, SEQ, D_MODEL), f32),
        "g_pre_mix": gain(ks[1], (DEPTH, D_MODEL)),
        "w_in": jax.random.normal(ks[2], (DEPTH, D_MODEL, D_IN), f32) * D_MODEL ** -0.5,
        "sgu_ln_g": gain(ks[3], (DEPTH, D_A)),
        "sgu_ln_b": 0.01 * jax.random.normal(ks[4], (DEPTH, D_A), f32),
        "sgu_w": jax.random.normal(ks[5], (DEPTH, SGU_GROUPS, SGU_BLOCK, SGU_BLOCK), f32) * SGU_BLOCK ** -0.5,
        "sgu_b": gain(ks[6], (DEPTH, SGU_GROUPS, SGU_BLOCK)),
        "rel_bias": 0.5 * jax.random.normal(ks[7], (NUM_BUCKETS, N_HEADS_B), f32),
        "w_o": jax.random.normal(ks[8], (DEPTH, D_MIX, D_MODEL), f32) * D_MIX ** -0.5,
        "g_post_mix": gain(ks[9], (DEPTH, D_MODEL)),
        "g_pre_ffn": gain(ks[10], (DEPTH, D_MODEL)),
        "w_gate": jax.random.normal(ks[11], (DEPTH, D_MODEL, D_FF), f32) * D_MODEL ** -0.5,
        "w_up": jax.random.normal(ks[12], (DEPTH, D_MODEL, D_FF), f32) * D_MODEL ** -0.5,
        "w_down": jax.random.normal(ks[13], (DEPTH, D_FF, D_MODEL), f32) * D_FF ** -0.5,
        "g_post_ffn": gain(ks[14], (DEPTH, D_MODEL)),
    }


def reference(x, g_pre_mix, w_in, sgu_ln_g, sgu_ln_b, sgu_w, sgu_b, rel_bias,
              w_o, g_post_mix, g_pre_ffn, w_gate, w_up, w_down, g_post_ffn):
    B, S, _ = x.shape
    offsets = np.cumsum(SPLITS)[:-1].tolist()
    for l in range(DEPTH):
        h = _rmsnorm(x, g_pre_mix[l])
        proj = h @ w_in[l]
        a_u, a_v, q, k, v, iq, ik, iw = jnp.split(proj, offsets, axis=-1)
        out_a = _spatial_gating(a_u, a_v, sgu_ln_g[l], sgu_ln_b[l], sgu_w[l], sgu_b[l])
        out_b = _dsa_attention(
            q.reshape(B, S, N_HEADS_B, HEAD_DIM_B),
            k.reshape(B, S, N_HEADS_B, HEAD_DIM_B),
            v.reshape(B, S, N_HEADS_B, HEAD_DIM_B),
            iq.reshape(B, S, IDX_HEADS, IDX_DIM), ik, iw, rel_bias)
        mix = jnp.concatenate([out_a, out_b], axis=-1) @ w_o[l]
        x = x + _rmsnorm(mix, g_post_mix[l])
        h = _rmsnorm(x, g_pre_ffn[l])
        f = (jax.nn.silu(h @ w_gate[l]) * (h @ w_up[l])) @ w_down[l]
        x = x + _rmsnorm(f, g_post_ffn[l])
    return x
```

```python
import numpy as np
from contextlib import ExitStack
import concourse.bass as bass
import concourse.mybir as mybir
from concourse.bass_utils import run_bass_kernel_spmd
from concourse.alu_op_type import AluOpType as ALU

F32 = mybir.dt.float32
BF16 = mybir.dt.bfloat16
AF = mybir.ActivationFunctionType
AX = mybir.AxisListType

D_MODEL = 1024
SEQ = 4096
D_FF = 2816
NFF = D_FF // 128
TOPK = 256
T_BIS = 16
EPS = 1e-6
NEG = -1.0e30
QSCALE = 128 ** -0.5
ISCALE = (64 ** -0.5) * (8 ** -0.5)


class _Stop(Exception):
    pass


class Sched:
    def __init__(self, nc, es):
        self.nc = nc
        self.es = es
        self.eng = {"pe": nc.tensor, "act": nc.scalar, "dve": nc.vector, "pool": nc.gpsimd, "sp": nc.sync}
        self.semh = {}
        self.cnt = {}
        for e in self.eng:
            self.semh[e] = es.enter_context(nc.semaphore("sem_" + e))
            self.cnt[e] = 0
        self.seen = {e: {} for e in self.eng}
        self.lastw = {}
        self.readers = {}
        self.nwait = 0
        self.nops = 0
        self.stop_ops = None
        self.log = None

    def _dma_key(self, slot):
        key = "dma:" + slot
        if key not in self.semh:
            self.semh[key] = self.es.enter_context(self.nc.semaphore("sd_" + slot))
            self.cnt[key] = 0
        return key

    PSUM_RES = frozenset(["MM0", "MM1", "MM2", "S0", "OB0", "OB1", "T0", "T1"])

    def op(self, eng, fn, reads=(), writes=(), dma=None):
        pr = [r for r in reads if r in self.PSUM_RES]
        if pr:
            writes = list(writes) + pr
        deps = {}

        def add(d, raw):
            if d is None:
                return
            key, val, deng = d
            if deng == eng and not key.startswith("dma:"):
                if eng in ("pe", "sp"):
                    return
            if deps.get(key, 0) < val:
                deps[key] = val

        for r in reads:
            add(self.lastw.get(r), True)
        for w in writes:
            add(self.lastw.get(w), False)
            for key, (val, deng) in self.readers.get(w, {}).items():
                add((key, val, deng), False)
        E = self.eng[eng]
        for key, val in deps.items():
            if self.seen[eng].get(key, 0) < val:
                E.wait_ge(self.semh[key], val)
                self.seen[eng][key] = val
                self.nwait += 1
        if self.stop_ops is not None and self.nops >= self.stop_ops:
            raise _Stop()
        inst = fn(E)
        if self.log is not None:
            import sys as _s
            self.log.append((self.nops, eng, _s._getframe(1).f_lineno, tuple(reads), tuple(writes)))
        self.nops += 1
        if dma is not None:
            key = self._dma_key(dma)
            self.cnt[key] += 16
            inst.then_inc(self.semh[key], 16)
        else:
            key = eng
            self.cnt[key] += 1
            inst.then_inc(self.semh[key], 1)
        val = self.cnt[key]
        for r in reads:
            d = self.readers.setdefault(r, {})
            if d.get(key, (0, None))[0] < val:
                d[key] = (val, eng)
        for w in writes:
            self.lastw[w] = (key, val, eng)
            self.readers[w] = {}
        return inst

    def barrier(self):
        for e in self.eng:
            E = self.eng[e]
            for key, val in self.cnt.items():
                if val > 0 and key != e and self.seen[e].get(key, 0) < val:
                    E.wait_ge(self.semh[key], val)
                    self.seen[e][key] = val

    def final_wait(self, eng="sp"):
        E = self.eng[eng]
        for key, val in self.cnt.items():
            if val > 0 and key != eng and self.seen[eng].get(key, 0) < val:
                E.wait_ge(self.semh[key], val)
                self.seen[eng][key] = val


def skew_emit(items, nstage):
    n = len(items)
    for step in range(n + nstage - 1):
        for s in range(nstage):
            j = step - s
            if 0 <= j < n:
                items[j][s]()


def build(NB=32, debug=False, stop=None, stop_ops=None):
    nc = bass.Bass("TRN2", target_bir_lowering=False)
    es = ExitStack()
    sc = Sched(nc, es)
    sc.stop_ops = stop_ops
    try:
        _build_body(nc, es, sc, NB, debug, stop)
    except _Stop:
        pass
    sc.final_wait("sp")
    es.close()
    nc._sched_stats = (sc.nops, sc.nwait)
    return nc


def _build_body(nc, es, sc, NB, debug, stop):
    def check(tag):
        if stop == tag:
            raise _Stop()

    S = NB * 128
    NG = NB // 4
    dt = nc.dram_tensor
    x_d = dt("x", [S, D_MODEL], F32, kind="ExternalInput").ap()
    w_in_d = dt("w_in", [D_MODEL, 3144], F32, kind="ExternalInput").ap()
    w_o_d = dt("w_o", [D_MODEL, D_MODEL], F32, kind="ExternalInput").ap()
    w_g_d = dt("w_gate", [D_MODEL, D_FF], F32, kind="ExternalInput").ap()
    w_u_d = dt("w_up", [D_MODEL, D_FF], F32, kind="ExternalInput").ap()
    w_d_d = dt("w_down", [D_FF, D_MODEL], F32, kind="ExternalInput").ap()
    ident_d = dt("ident", [128, 128], F32, kind="ExternalInput").ap()
    vis_d = dt("vis", [128, 128], F32, kind="ExternalInput").ap()
    pw_d = dt("pw", [128, T_BIS + 1], F32, kind="ExternalInput").ap()
    gcol_d = dt("gcol", [128, 16], F32, kind="ExternalInput").ap()
    gpost_d = dt("gpost", [128, 2048], F32, kind="ExternalInput").ap()
    ln_d = dt("lngb", [128, 1024], F32, kind="ExternalInput").ap()
    bh_d = dt("bh", [128, 4 * 256 + 4], F32, kind="ExternalInput").ap()
    wsT_d = dt("wsT", [128, 512], F32, kind="ExternalInput").ap()
    bs_d = dt("bsT", [128, 4], F32, kind="ExternalInput").ap()
    y_d = dt("y", [S, D_MODEL], F32, kind="ExternalOutput").ap()
    if debug:
        dbg_ob = dt("dbg_ob", [S, 512], BF16, kind="ExternalOutput").ap()
        dbg_sc = dt("dbg_sc", [128, 4096], F32, kind="ExternalOutput").ap()
        dbg_thr = dt("dbg_thr", [128, NB], F32, kind="ExternalOutput").ap()
        dbg_x1 = dt("dbg_x1", [S, D_MODEL], F32, kind="ExternalOutput").ap()

    op = sc.op

    def sb(name, shape, dtype, stack=es):
        return stack.enter_context(nc.sbuf_tensor("s_" + name, shape, dtype))

    def ps(name, shape, dtype, stack=es):
        return stack.enter_context(nc.psum_tensor("p_" + name, shape, dtype))

    ARENA_BYTES = 198 * 1024
    arena = es.enter_context(nc.sbuf_tensor("s_arena", [128, ARENA_BYTES // 2], BF16))
    apos = [0]

    def aset(off_bytes):
        apos[0] = off_bytes

    def av(name, shape, dtype, stack=None):
        esz = 4 if dtype == F32 else 2
        nel = 1
        for d in shape[1:]:
            nel *= d
        nbytes = (nel * esz + 63) // 64 * 64
        off = apos[0]
        assert off % 64 == 0 and off + nbytes <= ARENA_BYTES, (name, off, nbytes)
        apos[0] = off + nbytes
        v = arena[:, off // 2:off // 2 + nel * esz // 2]
        if dtype == F32:
            v = v.bitcast(F32)
        if len(shape) == 3:
            v = v.rearrange("p (a b) -> p a b", a=shape[1])
        elif len(shape) == 4:
            v = v.rearrange("p (a b c) -> p a b c", a=shape[1], b=shape[2])
        return v

    T0 = ps("T0", [128, 1024], BF16)
    T1 = ps("T1", [128, 1024], BF16)
    MM = [ps("MM%d" % i, [128, 512], F32) for i in range(3)]
    S0 = ps("S0", [128, 512], F32)
    OB = [ps("OB%d" % i, [128, 512], F32) for i in range(2)]
    TB = [T0, T1]

    ident = sb("ident", [128, 128], BF16)
    gcol = sb("gcol", [128, 16], F32)
    junk = sb("junk", [128, 1024], BF16)
    ss = sb("ss", [128, 8], F32)
    rstd = sb("rstd", [128, 8], F32)
    hb = sb("hb", [128, 1024], BF16)
    epsb = sb("epsb", [128, 2], F32)
    aset(0)
    outb = av("outb", [128, 32, 512], BF16)
    xs = av("xs", [128, 1024], F32)
    hT = av("hT", [128, 1024], BF16)
    P1A_BASE = 38 * 1024
    aset(P1A_BASE)
    sb = av
    p1a = None
    vis = sb("vis", [128, 128], F32, p1a)
    pw = sb("pw", [128, T_BIS + 1], F32, p1a)
    bh = sb("bh", [128, 4 * 256 + 4], F32, p1a)

    op("pool", lambda e: e.dma_start(out=ident[:], in_=ident_d[:, :]), writes=["ident"], dma="c_ident")
    op("sp", lambda e: e.dma_start(out=vis[:], in_=vis_d[:, :]), writes=["vis"], dma="c0")
    op("sp", lambda e: e.dma_start(out=pw[:], in_=pw_d[:, :]), writes=["pw"], dma="c1")
    op("sp", lambda e: e.dma_start(out=gcol[:], in_=gcol_d[:, :]), writes=["gcol"], dma="c2")
    op("sp", lambda e: e.dma_start(out=bh[:], in_=bh_d[:, :]), writes=["bh"], dma="c4")

    def rms_rstd(src_ops, col):
        op("act", lambda e: e.activation(out=rstd[:, col:col + 1], in_=ss[:, col:col + 1], func=AF.Ln,
                                         bias=epsb[:, 0:1], scale=1.0 / 1024),
           reads=["ss%d" % col, "epsb"], writes=["rstd%d" % col])
        op("act", lambda e: e.activation(out=rstd[:, col:col + 1], in_=rstd[:, col:col + 1], func=AF.Exp,
                                         scale=-0.5),
           reads=["rstd%d" % col], writes=["rstd%d" % col])

    op("dve", lambda e: e.memset(epsb[:, 0:1], EPS), writes=["epsb"])
    check("consts")

    def load_norm_transpose(i, gc0):
        t0 = i * 128
        op("sp", lambda e: e.dma_start(out=xs[:], in_=x_d[t0:t0 + 128, :]), writes=["xs"], dma="xs")
        op("act", lambda e: e.activation(out=junk[:, 0:1024], in_=xs[:], func=AF.Square, accum_out=ss[:, 0:1]),
           reads=["xs"], writes=["ss0", "junk"])
        rms_rstd(None, 0)
        op("dve", lambda e: e.tensor_scalar(out=hb[:], in0=xs[:], scalar1=rstd[:, 0:1], scalar2=None, op0=ALU.mult),
           reads=["xs", "rstd0"], writes=["hb"])
        for k in range(8):
            op("pe", lambda e, k=k: e.transpose(out=T0[:, 128 * k:128 * k + 128], in_=hb[:, 128 * k:128 * k + 128],
                                                identity=ident[:]),
               reads=["hb", "ident"], writes=["T0"])
        for k in range(8):
            if True:
                op("dve", lambda e, k=k: e.tensor_scalar(out=hT[:, 128 * k:128 * k + 128], in0=T0[:, 128 * k:128 * k + 128],
                                                         scalar1=gcol[:, gc0 + k:gc0 + k + 1], scalar2=None, op0=ALU.mult),
                   reads=["T0", "gcol"], writes=["hT%d" % k])
            else:
                op("act", lambda e, k=k: e.activation(out=hT[:, 128 * k:128 * k + 128], in_=T0[:, 128 * k:128 * k + 128],
                                                      func=AF.Identity, scale=gcol[:, gc0 + k:gc0 + k + 1]),
                   reads=["T0", "gcol"], writes=["hT%d" % k])

    w1 = sb("w1", [128, 8, 2120], BF16, p1a)
    kT = sb("kT", [128, 4, S], BF16, p1a)
    vc = sb("vc", [128, NB, 4, 130], BF16, p1a)
    ikT = sb("ikT", [128, S], BF16, p1a)
    score = sb("score", [128, S], F32, p1a)
    sel = sb("sel", [128, S], BF16, p1a)
    q_sb = sb("q_sb", [128, 512], BF16, p1a)
    k_sb = sb("k_sb", [128, 512], BF16, p1a)
    iq_sb = sb("iq_sb", [128, 512], BF16, p1a)
    ik2 = sb("ik2", [128, 128], BF16, p1a)
    iwc = sb("iwc", [128, 8], F32, p1a)
    qT = sb("qT", [128, 512], BF16, p1a)
    iqT = sb("iqT", [128, 512], BF16, p1a)
    Dg = sb("Dg", [128, 8, 128], BF16, p1a)
    rbuf = [sb("r%d" % j, [128, 512], BF16, p1a) for j in range(3)]
    Eb = [sb("E%d" % j, [128, 512], BF16, p1a) for j in range(2)]
    Pb = [sb("P%d" % j, [128, 512], BF16, p1a) for j in range(2)]
    PTb = [sb("PT%d" % j, [128, 512], BF16, p1a) for j in range(2)]
    tmpb = sb("tmpb", [128, 256], F32, p1a)
    amaxp = sb("amaxp", [128, 8], F32, p1a)
    aminp = sb("aminp", [128, 8], F32, p1a)
    bis = sb("bis", [128, 8], F32, p1a)
    D2 = sb("D2", [128, T_BIS + 1], F32, p1a)
    thr_all = sb("thr_all", [128, NB], F32, p1a)

    for k in range(8):
        op("pool", lambda e, k=k: e.dma_start(out=w1[:, k, :], in_=w_in_d[128 * k:128 * k + 128, 1024:3144],
                                              max_dma_last_dim=4096),
           writes=["w1_%d" % k], dma="w1_%d" % k)
    op("dve", lambda e: e.memset(vc[:, :, :, 128:130], 1.0), writes=["vc_ones"])

    dots_rot = [0]
    lg_rot = [0]
    e_rot = [0]
    tb_rot = [0]

    for i in range(NB):
        t0 = i * 128
        L = t0 + 128
        load_norm_transpose(i, 0)
        groups = [("q", 0, 512), ("k", 512, 512), ("v", 1024, 512), ("iq", 1536, 512), ("ik", 2048, 72)]
        for gi, (nm, c0, w) in enumerate(groups):
            bank = MM[gi % 3]
            bname = "MM%d" % (gi % 3)
            for k in range(8):
                op("pe", lambda e, k=k, bank=bank, c0=c0, w=w: e.matmul(
                    bank[:, 0:w], lhsT=hT[:, 128 * k:128 * k + 128], rhs=w1[:, k, c0:c0 + w],
                    start=(k == 0), stop=(k == 7)),
                   reads=["hT%d" % k, "w1_%d" % k], writes=[bname])
            if nm == "q":
                op("act", lambda e, bank=bank: e.activation(out=q_sb[:], in_=bank[:, :], func=AF.Copy),
                   reads=[bname], writes=["q_sb"])
            elif nm == "k":
                op("dve", lambda e, bank=bank: e.tensor_copy(out=k_sb[:], in_=bank[:, :]),
                   reads=[bname], writes=["k_sb"])
            elif nm == "v":
                op("act", lambda e, bank=bank, i=i: e.activation(
                    out=vc[:, i, :, 0:128], in_=bank[:, :].rearrange("p (h d) -> p h d", h=4), func=AF.Copy),
                   reads=[bname], writes=["vc:%d" % i])
            elif nm == "iq":
                op("dve", lambda e, bank=bank: e.tensor_copy(out=iq_sb[:], in_=bank[:, :]),
                   reads=[bname], writes=["iq_sb"])
            else:
                op("act", lambda e, bank=bank: e.activation(out=ik2[:, 0:64], in_=bank[:, 0:64], func=AF.Copy),
                   reads=[bname], writes=["ik2a"])
                op("dve", lambda e, bank=bank: e.tensor_copy(out=ik2[:, 64:128], in_=bank[:, 0:64]),
                   reads=[bname], writes=["ik2b"])
                op("dve", lambda e, bank=bank: e.tensor_scalar(out=iwc[:], in0=bank[:, 64:72], scalar1=ISCALE,
                                                               scalar2=None, op0=ALU.mult),
                   reads=[bname], writes=["iwc"])
        for h in range(4):
            op("pe", lambda e, h=h: e.transpose(out=T1[:, 128 * h:128 * h + 128], in_=q_sb[:, 128 * h:128 * h + 128],
                                                identity=ident[:]),
               reads=["q_sb", "ident"], writes=["T1"])
        for h in range(4):
            op("pe", lambda e, h=h: e.transpose(out=T1[:, 512 + 128 * h:512 + 128 * h + 128],
                                                in_=k_sb[:, 128 * h:128 * h + 128], identity=ident[:]),
               reads=["k_sb", "ident"], writes=["T1"])
        op("dve", lambda e: e.tensor_copy(out=qT[:], in_=T1[:, 0:512]), reads=["T1"], writes=["qT"])
        op("dve", lambda e, t0=t0: e.tensor_copy(out=kT[:, :, t0:t0 + 128],
                                                 in_=T1[:, 512:1024].rearrange("p (h t) -> p h t", h=4)),
           reads=["T1"], writes=["kT:%d" % i])
        for h in range(4):
            op("pe", lambda e, h=h: e.transpose(out=T0[:, 128 * h:128 * h + 128], in_=iq_sb[:, 128 * h:128 * h + 128],
                                                identity=ident[:]),
               reads=["iq_sb", "ident"], writes=["T0"])
        op("pe", lambda e: e.transpose(out=T0[:, 512:640], in_=ik2[:, :], identity=ident[:]),
           reads=["ik2a", "ik2b", "ident"], writes=["T0"])
        op("dve", lambda e: e.tensor_copy(out=iqT[:], in_=T0[:, 0:512]), reads=["T0"], writes=["iqT"])
        op("dve", lambda e, t0=t0: e.tensor_copy(out=ikT[:, t0:t0 + 128], in_=T0[:, 512:640]),
           reads=["T0"], writes=["ikT:%d" % i])
        for h in range(8):
            op("dve", lambda e, h=h: e.tensor_scalar(out=Dg[:, h, :], in0=ident[:], scalar1=iwc[:, h:h + 1],
                                                      scalar2=1.0, op0=ALU.mult, op1=ALU.mult),
               reads=["ident", "iwc"], writes=["Dg%d" % h])

        check("A%d" % i)
        nkt = (L + 511) // 512
        pairs = [(kt, h) for kt in range(nkt) for h in range(8)]

        def emit_dots(n):
            kt, h = pairs[n]
            N = min(512, L - 512 * kt)
            b = dots_rot[0] % 3
            dots_rot[0] += 1
            pb = 64 * (h % 2)
            cb = 128 * (h // 2)
            blks = ["ikT:%d" % bb for bb in range(4 * kt, min(4 * kt + 4, i + 1))]
            op("pe", lambda e: e.matmul(MM[b][:, 0:N], lhsT=iqT[pb:pb + 64, cb:cb + 128],
                                        rhs=ikT[pb:pb + 64, 512 * kt:512 * kt + N], start=True, stop=True),
               reads=["iqT"] + blks, writes=["MM%d" % b])
            return b

        dbank = {}
        for n in range(min(3, len(pairs))):
            dbank[n] = emit_dots(n)
        for n, (kt, h) in enumerate(pairs):
            N = min(512, L - 512 * kt)
            b = dbank[n]
            rj = n % 3
            op("act", lambda e, b=b, rj=rj, N=N: e.activation(out=rbuf[rj][:, 0:N], in_=MM[b][:, 0:N], func=AF.Relu),
               reads=["MM%d" % b], writes=["r%d" % rj])
            op("pe", lambda e, rj=rj, N=N, h=h: e.matmul(S0[:, 0:N], lhsT=Dg[:, h, :], rhs=rbuf[rj][:, 0:N],
                                                         start=(h == 0), stop=(h == 7)),
               reads=["r%d" % rj, "Dg%d" % h], writes=["S0"])
            if n + 3 < len(pairs):
                dbank[n + 3] = emit_dots(n + 3)
            if h == 7:
                op("act", lambda e, kt=kt, N=N: e.activation(out=score[:, 512 * kt:512 * kt + N], in_=S0[:, 0:N],
                                                             func=AF.Copy),
                   reads=["S0"], writes=["score"])
                op("dve", lambda e, kt=kt, N=N: e.tensor_scalar(
                    out=sel[:, 512 * kt:512 * kt + N], in0=score[:, 512 * kt:512 * kt + N], scalar1=-3.0e38, scalar2=None,
                    op0=ALU.max, op1=ALU.max, accum_out=amaxp[:, kt:kt + 1]),
                   reads=["score"], writes=["amaxp", "sel"])
                op("dve", lambda e, kt=kt, N=N: e.tensor_scalar(
                    out=sel[:, 512 * kt:512 * kt + N], in0=score[:, 512 * kt:512 * kt + N], scalar1=3.0e38, scalar2=None,
                    op0=ALU.min, op1=ALU.min, accum_out=aminp[:, kt:kt + 1]),
                   reads=["score"], writes=["aminp", "sel"])
        op("dve", lambda e, L=L: e.tensor_tensor(out=score[:, L - 128:L], in0=score[:, L - 128:L], in1=vis[:],
                                                  op=ALU.add),
           reads=["score", "vis"], writes=["score"])

        check("I%d" % i)
        A_, MID, CNT, PM, THR = (bis[:, j:j + 1] for j in range(5))
        if L > TOPK:
            op("dve", lambda e: e.tensor_scalar(out=amaxp[:, 0:nkt], in0=amaxp[:, 0:nkt], scalar1=-3.0e38, scalar2=None,
                                                op0=ALU.max, op1=ALU.max, accum_out=bis[:, 6:7]),
               reads=["amaxp"], writes=["amaxp", "bis6"])
            op("dve", lambda e: e.tensor_scalar(out=aminp[:, 0:nkt], in0=aminp[:, 0:nkt], scalar1=3.0e38, scalar2=None,
                                                op0=ALU.min, op1=ALU.min, accum_out=bis[:, 7:8]),
               reads=["aminp"], writes=["aminp", "bis7"])
            op("dve", lambda e: e.scalar_tensor_tensor(out=A_, in0=bis[:, 7:8], scalar=-1.0, in1=bis[:, 6:7],
                                                       op0=ALU.mult, op1=ALU.max),
               reads=["bis6", "bis7"], writes=["bisA"])
            op("dve", lambda e: e.tensor_scalar(out=D2[:], in0=pw[:], scalar1=A_, scalar2=None, op0=ALU.mult),
               reads=["bisA", "pw"], writes=["D2"])
            op("dve", lambda e: e.memset(MID, 0.0), writes=["mid"])
            for k in range(T_BIS):
                op("dve", lambda e: e.tensor_scalar(out=sel[:, 0:L], in0=score[:, 0:L], scalar1=MID, scalar2=None,
                                                    op0=ALU.is_gt, op1=ALU.add, accum_out=CNT),
                   reads=["score", "mid"], writes=["cnt", "sel"])
                op("dve", lambda e: e.tensor_scalar(out=PM, in0=CNT, scalar1=TOPK - 0.5, scalar2=0.5,
                                                    op0=ALU.is_gt, op1=ALU.subtract),
                   reads=["cnt"], writes=["pm"])
                op("dve", lambda e, k=k: e.scalar_tensor_tensor(out=MID, in0=PM, scalar=D2[:, k:k + 1], in1=MID,
                                                                op0=ALU.mult, op1=ALU.add),
                   reads=["pm", "D2", "mid"], writes=["mid"])
            op("dve", lambda e: e.tensor_tensor(out=THR, in0=MID, in1=D2[:, T_BIS:T_BIS + 1], op=ALU.subtract),
               reads=["mid", "D2"], writes=["thr"])
        else:
            op("dve", lambda e: e.memset(THR, -1.0e29), writes=["thr"])
        if debug:
            op("dve", lambda e, i=i: e.tensor_copy(out=thr_all[:, i:i + 1], in_=THR), reads=["thr"], writes=["thr_all"])
        op("dve", lambda e, L=L: e.tensor_scalar(out=sel[:, 0:L], in0=score[:, 0:L], scalar1=THR, scalar2=None,
                                                 op0=ALU.is_gt),
           reads=["score", "thr"], writes=["sel"])

        check("B%d" % i)
        ntile = (L + 511) // 512
        bw = min(256, L)
        items = []
        for h in range(4):
            for j in range(ntile - 1, -1, -1):
                c1 = L - 512 * j
                c0 = max(0, c1 - 512)
                N = c1 - c0
                first = (j == ntile - 1)
                last = (j == 0)
                st = {}

                def s1(h=h, c0=c0, c1=c1, N=N, j=j, st=st):
                    b = lg_rot[0] % 3
                    lg_rot[0] += 1
                    ej = e_rot[0] % 2
                    e_rot[0] += 1
                    st["ej"] = ej
                    blks = ["kT:%d" % bb for bb in range(c0 // 128, c1 // 128)]
                    op("pe", lambda e: e.matmul(MM[b][:, 0:N], lhsT=qT[:, 128 * h:128 * h + 128], rhs=kT[:, h, c0:c1],
                                                start=True, stop=True),
                       reads=["qT"] + blks, writes=["MM%d" % b])
                    nb_ = bw if j == 0 else 0
                    nf = N - nb_
                    if nf > 0:
                        op("act", lambda e: e.activation(out=Eb[ej][:, 0:nf], in_=MM[b][:, 0:nf], func=AF.Exp,
                                                         bias=bh[:, 1024 + h:1025 + h], scale=QSCALE),
                           reads=["MM%d" % b, "bh"], writes=["E%d" % ej])
                    if nb_ > 0:
                        op("dve", lambda e: e.scalar_tensor_tensor(
                            out=tmpb[:, 0:nb_], in0=MM[b][:, nf:N], scalar=QSCALE,
                            in1=bh[:, 256 * h + 256 - nb_:256 * h + 256], op0=ALU.mult, op1=ALU.add),
                           reads=["MM%d" % b, "bh"], writes=["tmpb"])
                        op("act", lambda e: e.activation(out=Eb[ej][:, nf:N], in_=tmpb[:, 0:nb_], func=AF.Exp),
                           reads=["tmpb"], writes=["E%d" % ej])
                    op("dve", lambda e: e.tensor_tensor(out=Pb[ej][:, 0:N], in0=Eb[ej][:, 0:N], in1=sel[:, c0:c1],
                                                        op=ALU.mult),
                       reads=["E%d" % ej, "sel"], writes=["P%d" % ej])

                def s2(N=N, st=st):
                    ej = st["ej"]
                    tb = tb_rot[0] % 2
                    tb_rot[0] += 1
                    st["tb"] = tb
                    for m in range(N // 128):
                        op("pe", lambda e, m=m: e.transpose(out=TB[tb][:, 128 * m:128 * m + 128],
                                                            in_=Pb[ej][:, 128 * m:128 * m + 128], identity=ident[:]),
                           reads=["P%d" % ej, "ident"], writes=["T%d" % tb])
                    eng = "dve"
                    if eng == "act":
                        op("act", lambda e: e.activation(out=PTb[tb][:, 0:N], in_=TB[tb][:, 0:N], func=AF.Copy),
                           reads=["T%d" % tb], writes=["PT%d" % tb])
                    else:
                        op("dve", lambda e: e.tensor_copy(out=PTb[tb][:, 0:N], in_=TB[tb][:, 0:N]),
                           reads=["T%d" % tb], writes=["PT%d" % tb])

                def s3(h=h, c0=c0, N=N, first=first, last=last, st=st, i=i):
                    tb = st["tb"]
                    ob = OB[h // 2]
                    off = 130 * (h % 2)
                    nm = N // 128
                    for m in range(nm):
                        blk = c0 // 128 + m
                        op("pe", lambda e, m=m, blk=blk: e.matmul(
                            ob[:, off:off + 130], lhsT=PTb[tb][:, 128 * m:128 * m + 128], rhs=vc[:, blk, h, :],
                            start=(first and m == 0), stop=(last and m == nm - 1)),
                           reads=["PT%d" % tb, "vc:%d" % blk, "vc_ones"], writes=["OB%d" % (h // 2)])
                    if last:
                        RS = bis[:, 5:6]
                        op("dve", lambda e: e.reciprocal(out=RS, in_=ob[:, off + 128:off + 129]),
                           reads=["OB%d" % (h // 2)], writes=["rs"])
                        op("dve", lambda e: e.tensor_scalar(out=outb[:, i, 128 * h:128 * h + 128], in0=ob[:, off:off + 128],
                                                            scalar1=RS, scalar2=None, op0=ALU.mult),
                           reads=["OB%d" % (h // 2), "rs"], writes=["outb:%d" % i])

                items.append([s1, s2, s3])
        skew_emit(items, 3)
        check("C%d" % i)

    if debug:
        op("sp", lambda e: e.dma_start(out=dbg_sc[:, 0:S], in_=score[:, 0:S]), reads=["score"], dma="dbg0")
        op("sp", lambda e: e.dma_start(out=dbg_thr[:, :], in_=thr_all[:]), reads=["thr_all"], dma="dbg1")
        for i in range(NB):
            op("sp", lambda e, i=i: e.dma_start(out=dbg_ob[128 * i:128 * i + 128, :], in_=outb[:, i, :]),
               reads=["outb:%d" % i], dma="dbg2")
    sc.barrier()

    check("P1a")
    aset(P1A_BASE)
    wg = sb("wg", [128, 8, D_FF], BF16)
    wu = sb("wu", [128, 8, D_FF], BF16)
    P1B_BASE = apos[0]
    p1b = None
    w1b = sb("w1b", [128, 8, 1024], BF16, p1b)
    wo = sb("wo", [128, 8, 1024], BF16, p1b)
    lngb = sb("lngb", [128, 1024], F32, p1b)
    wsT = sb("wsT", [128, 512], BF16, p1b)
    bsT = sb("bsT", [128, 4], F32, p1b)
    u_sb = sb("u_sb", [128, 512], F32, p1b)
    avb = sb("av", [128, 512], F32, p1b)
    vn = sb("vn", [128, 512], BF16, p1b)
    cata = sb("cata", [128, 512], BF16, p1b)
    catT = sb("catT", [128, 1024], BF16, p1b)
    tmpx = sb("tmpx", [128, 1024], F32, p1b)
    bst = sb("bst", [128, 8], F32, p1b)
    mv = sb("mv", [128, 8], F32, p1b)
    gpost = sb("gpost1", [128, 1024], F32, p1b)
    op("sp", lambda e: e.dma_start(out=gpost[:], in_=gpost_d[:, 0:1024]), writes=["gpost"], dma="c3")

    for k in range(8):
        op("pool", lambda e, k=k: e.dma_start(out=w1b[:, k, :], in_=w_in_d[128 * k:128 * k + 128, 0:1024]),
           writes=["w1b_%d" % k], dma="w1b_%d" % k)
    for k in range(8):
        op("pool", lambda e, k=k: e.dma_start(out=wo[:, k, :], in_=w_o_d[128 * k:128 * k + 128, :]),
           writes=["wo_%d" % k], dma="wo_%d" % k)
    op("sp", lambda e: e.dma_start(out=lngb[:], in_=ln_d[:, :]), writes=["lngb"], dma="c0")
    op("pool", lambda e: e.dma_start(out=wsT[:], in_=wsT_d[:, :]), writes=["wsT"], dma="c_ident")
    op("sp", lambda e: e.dma_start(out=bsT[:], in_=bs_d[:, :]), writes=["bsT"], dma="c1")
    for g in range(4):
        op("dve", lambda e, g=g: e.memset(wsT[64:128, 128 * g:128 * g + 64], 0.0), reads=["wsT"], writes=["wsT"])

    ffn_loads = []
    for k in range(8):
        for half in range(2):
            c0 = half * 1408
            ffn_loads.append(lambda k=k, c0=c0, half=half: op(
                "pool", lambda e: e.dma_start(out=wg[:, k, c0:c0 + 1408], in_=w_g_d[128 * k:128 * k + 128, c0:c0 + 1408]),
                writes=["wg_%d_%d" % (k, half)], dma="wg_%d_%d" % (k, c0)))
            ffn_loads.append(lambda k=k, c0=c0, half=half: op(
                "pool", lambda e: e.dma_start(out=wu[:, k, c0:c0 + 1408], in_=w_u_d[128 * k:128 * k + 128, c0:c0 + 1408]),
                writes=["wu_%d_%d" % (k, half)], dma="wu_%d_%d" % (k, c0)))

    for i in range(NB):
        t0 = i * 128
        load_norm_transpose(i, 0)
        for gi in range(2):
            bank = MM[gi]
            for k in range(8):
                op("pe", lambda e, k=k, bank=bank, gi=gi: e.matmul(
                    bank[:, :], lhsT=hT[:, 128 * k:128 * k + 128], rhs=w1b[:, k, 512 * gi:512 * gi + 512],
                    start=(k == 0), stop=(k == 7)),
                   reads=["hT%d" % k, "w1b_%d" % k], writes=["MM%d" % gi])
        op("act", lambda e: e.activation(out=u_sb[:], in_=MM[0][:, :], func=AF.Copy), reads=["MM0"], writes=["u_sb"])
        op("dve", lambda e: e.tensor_copy(out=avb[:], in_=MM[1][:, :]), reads=["MM1"], writes=["av"])
        op("dve", lambda e: e.bn_stats(out=bst[:, 0:6], in_=avb[:]), reads=["av"], writes=["bst"])
        op("dve", lambda e: e.bn_aggr(out=mv[:, 0:2], in_=bst[:, 0:6]), reads=["bst"], writes=["mv"])
        op("act", lambda e: e.activation(out=mv[:, 2:3], in_=mv[:, 1:2], func=AF.Ln, bias=epsb[:, 0:1], scale=1.0),
           reads=["mv", "epsb"], writes=["mv2"])
        op("act", lambda e: e.activation(out=mv[:, 3:4], in_=mv[:, 2:3], func=AF.Exp, scale=-0.5),
           reads=["mv2"], writes=["mv3"])
        op("dve", lambda e: e.tensor_scalar(out=avb[:], in0=avb[:], scalar1=mv[:, 0:1], scalar2=mv[:, 3:4],
                                            op0=ALU.subtract, op1=ALU.mult),
           reads=["av", "mv", "mv3"], writes=["av"])
        op("dve", lambda e: e.tensor_tensor(out=avb[:], in0=avb[:], in1=lngb[:, 0:512], op=ALU.mult),
           reads=["av", "lngb"], writes=["av"])
        op("dve", lambda e: e.tensor_tensor(out=vn[:], in0=avb[:], in1=lngb[:, 512:1024], op=ALU.add),
           reads=["av", "lngb"], writes=["vn"])
        for g in range(4):
            op("pe", lambda e, g=g: e.matmul(MM[2][:, 128 * g:128 * g + 128], lhsT=wsT[:, 128 * g:128 * g + 128],
                                             rhs=vn[:, 128 * g:128 * g + 128], start=True, stop=True),
               reads=["wsT", "vn"], writes=["MM2"])
        for g in range(4):
            op("dve", lambda e, g=g: e.scalar_tensor_tensor(
                out=cata[:, 128 * g:128 * g + 128], in0=MM[2][:, 128 * g:128 * g + 128], scalar=bsT[:, g:g + 1],
                in1=u_sb[:, 128 * g:128 * g + 128], op0=ALU.add, op1=ALU.mult),
               reads=["MM2", "bsT", "u_sb"], writes=["cata"])
        for k in range(8):
            src = cata[:, 128 * k:128 * k + 128] if k < 4 else outb[:, i, 128 * (k - 4):128 * (k - 4) + 128]
            op("pe", lambda e, k=k, src=src: e.transpose(out=T1[:, 128 * k:128 * k + 128], in_=src, identity=ident[:]),
               reads=["cata", "outb:%d" % i, "ident"], writes=["T1"])
        op("dve", lambda e: e.tensor_copy(out=catT[:, 0:512], in_=T1[:, 0:512]), reads=["T1"], writes=["catTa"])
        op("dve", lambda e: e.tensor_copy(out=catT[:, 512:1024], in_=T1[:, 512:1024]), reads=["T1"], writes=["catTb"])
        for og in range(2):
            for k in range(8):
                op("pe", lambda e, k=k, og=og: e.matmul(
                    MM[og][:, :], lhsT=catT[:, 128 * k:128 * k + 128], rhs=wo[:, k, 512 * og:512 * og + 512],
                    start=(k == 0), stop=(k == 7)),
                   reads=["catTa" if k < 4 else "catTb", "wo_%d" % k], writes=["MM%d" % og])
        op("act", lambda e: e.activation(out=junk[:, 0:512], in_=MM[0][:, :], func=AF.Square, accum_out=ss[:, 2:3]),
           reads=["MM0"], writes=["ssa", "junk"])
        op("act", lambda e: e.activation(out=junk[:, 512:1024], in_=MM[1][:, :], func=AF.Square, accum_out=ss[:, 3:4]),
           reads=["MM1"], writes=["ssb", "junk"])
        op("dve", lambda e: e.tensor_tensor(out=ss[:, 1:2], in0=ss[:, 2:3], in1=ss[:, 3:4], op=ALU.add),
           reads=["ssa", "ssb"], writes=["ss1"])
        rms_rstd(None, 1)
        for og in range(2):
            op("dve", lambda e, og=og: e.scalar_tensor_tensor(
                out=tmpx[:, 512 * og:512 * og + 512], in0=MM[og][:, :], scalar=rstd[:, 1:2],
                in1=gpost[:, 512 * og:512 * og + 512], op0=ALU.mult, op1=ALU.mult),
               reads=["MM%d" % og, "rstd1", "gpost"], writes=["tmpx%d" % og])
        op("dve", lambda e: e.tensor_tensor(out=xs[:], in0=xs[:], in1=tmpx[:], op=ALU.add),
           reads=["xs", "tmpx0", "tmpx1"], writes=["xs"])
        op("sp", lambda e, t0=t0: e.dma_start(out=y_d[t0:t0 + 128, :], in_=xs[:]), reads=["xs"],
           writes=["y:%d" % i], dma="yst")
        if debug:
            op("sp", lambda e, t0=t0: e.dma_start(out=dbg_x1[t0:t0 + 128, :], in_=xs[:]), reads=["xs"], dma="dbg3")
        nper = (len(ffn_loads) + NB - 1) // NB
        for _ in range(nper):
            if ffn_loads:
                ffn_loads.pop(0)()
    while ffn_loads:
        ffn_loads.pop(0)()
    sc.barrier()

    check("P1b")
    p2 = None
    aset(P1B_BASE)
    wd = sb("wd", [128, NFF, D_MODEL], BF16, p2)
    aset(32 * 1024)
    gpost = sb("gpost2", [128, 1024], F32, p2)
    op("sp", lambda e: e.dma_start(out=gpost[:], in_=gpost_d[:, 1024:2048]), writes=["gpost"], dma="c3")
    aset(P1B_BASE + NFF * D_MODEL * 2)
    for c in range(NFF):
        op("pool", lambda e, c=c: e.dma_start(out=wd[:, c, :], in_=w_d_d[128 * c:128 * c + 128, :]),
           writes=["wd_%d" % c], dma="wd_%d" % c)
    actT = sb("actT", [128, NFF, 512], BF16, p2)
    aset(0)
    x1g = sb("x1g", [128, 4, 1024], F32, p2)
    h2T = sb("h2T", [128, 8, 512], BF16, p2)
    sg = [sb("sg%d" % j, [128, 512], F32, p2) for j in range(2)]
    outx = sb("outx", [128, 1024], F32, p2)
    assert apos[0] <= 32 * 1024
    GA = [MM[0], MM[1]]
    UB = [MM[2], S0]
    FB = [OB[0], OB[1]]
    GAn = ["MM0", "MM1"]
    UBn = ["MM2", "S0"]
    FBn = ["OB0", "OB1"]

    for g in range(NG):
        for b in range(4):
            i = 4 * g + b
            op("sp", lambda e, i=i, b=b: e.dma_start(out=x1g[:, b, :], in_=y_d[128 * i:128 * i + 128, :]),
               reads=["y:%d" % i], writes=["x1g%d" % b], dma="x1g%d" % b)
        for b in range(4):
            op("act", lambda e, b=b: e.activation(out=junk[:, 0:1024], in_=x1g[:, b, :], func=AF.Square,
                                                  accum_out=ss[:, 4 + b:5 + b]),
               reads=["x1g%d" % b], writes=["ss%d" % (4 + b), "junk"])
        for b in range(4):
            rms_rstd(None, 4 + b)
        for b in range(4):
            op("dve", lambda e, b=b: e.tensor_scalar(out=hb[:], in0=x1g[:, b, :], scalar1=rstd[:, 4 + b:5 + b],
                                                     scalar2=None, op0=ALU.mult),
               reads=["x1g%d" % b, "rstd%d" % (4 + b)], writes=["hb"])
            tb = b % 2
            for k in range(8):
                op("pe", lambda e, k=k, tb=tb: e.transpose(out=TB[tb][:, 128 * k:128 * k + 128],
                                                           in_=hb[:, 128 * k:128 * k + 128], identity=ident[:]),
                   reads=["hb", "ident"], writes=["T%d" % tb])
            for k in range(8):
                if True:
                    op("dve", lambda e, k=k, tb=tb, b=b: e.tensor_scalar(
                        out=h2T[:, k, 128 * b:128 * b + 128], in0=TB[tb][:, 128 * k:128 * k + 128],
                        scalar1=gcol[:, 8 + k:9 + k], scalar2=None, op0=ALU.mult),
                       reads=["T%d" % tb, "gcol"], writes=["h2T%d" % k])
                else:
                    op("act", lambda e, k=k, tb=tb, b=b: e.activation(
                        out=h2T[:, k, 128 * b:128 * b + 128], in_=TB[tb][:, 128 * k:128 * k + 128], func=AF.Identity,
                        scale=gcol[:, 8 + k:9 + k]),
                       reads=["T%d" % tb, "gcol"], writes=["h2T%d" % k])
        for c in range(NFF):
            j = c % 2
            for k in range(8):
                op("pe", lambda e, k=k, c=c, j=j: e.matmul(GA[j][:, :], lhsT=wg[:, k, 128 * c:128 * c + 128], rhs=h2T[:, k, :],
                                                           start=(k == 0), stop=(k == 7)),
                   reads=["wg_%d_%d" % (k, 0 if c < 11 else 1), "h2T%d" % k], writes=[GAn[j]])
            for k in range(8):
                op("pe", lambda e, k=k, c=c, j=j: e.matmul(UB[j][:, :], lhsT=wu[:, k, 128 * c:128 * c + 128], rhs=h2T[:, k, :],
                                                           start=(k == 0), stop=(k == 7)),
                   reads=["wu_%d_%d" % (k, 0 if c < 11 else 1), "h2T%d" % k], writes=[UBn[j]])
            op("act", lambda e, j=j: e.activation(out=sg[j][:], in_=GA[j][:, :], func=AF.Silu),
               reads=[GAn[j]], writes=["sg%d" % j])
            op("dve", lambda e, j=j, c=c: e.tensor_tensor(out=actT[:, c, :], in0=sg[j][:], in1=UB[j][:, :], op=ALU.mult),
               reads=["sg%d" % j, UBn[j]], writes=["actT%d" % c])
        for b in range(4):
            i = 4 * g + b
            for og in range(2):
                for c in range(NFF):
                    op("pe", lambda e, c=c, og=og, b=b: e.matmul(
                        FB[og][:, :], lhsT=actT[:, c, 128 * b:128 * b + 128], rhs=wd[:, c, 512 * og:512 * og + 512],
                        start=(c == 0), stop=(c == NFF - 1)),
                       reads=["actT%d" % c, "wd_%d" % c], writes=[FBn[og]])
            op("act", lambda e: e.activation(out=junk[:, 0:512], in_=FB[0][:, :], func=AF.Square, accum_out=ss[:, 2:3]),
               reads=["OB0"], writes=["ssa", "junk"])
            op("act", lambda e: e.activation(out=junk[:, 512:1024], in_=FB[1][:, :], func=AF.Square, accum_out=ss[:, 3:4]),
               reads=["OB1"], writes=["ssb", "junk"])
            op("dve", lambda e: e.tensor_tensor(out=ss[:, 1:2], in0=ss[:, 2:3], in1=ss[:, 3:4], op=ALU.add),
               reads=["ssa", "ssb"], writes=["ss1"])
            rms_rstd(None, 1)
            for og in range(2):
                op("dve", lambda e, og=og: e.scalar_tensor_tensor(
                    out=outx[:, 512 * og:512 * og + 512], in0=FB[og][:, :], scalar=rstd[:, 1:2],
                    in1=gpost[:, 512 * og:512 * og + 512], op0=ALU.mult, op1=ALU.mult),
                   reads=[FBn[og], "rstd1", "gpost"], writes=["outx%d" % og])
            op("dve", lambda e, b=b: e.tensor_tensor(out=outx[:], in0=outx[:], in1=x1g[:, b, :], op=ALU.add),
               reads=["outx0", "outx1", "x1g%d" % b], writes=["outx0", "outx1"])
            op("sp", lambda e, i=i: e.dma_start(out=y_d[128 * i:128 * i + 128, :], in_=outx[:]),
               reads=["outx0", "outx1"], writes=["y:%d" % i], dma="yst")
    return


def _t5_bucket(rel):
    rel = np.asarray(rel, np.int32)
    nb = 16
    ret = (rel > 0).astype(np.int32) * nb
    n = np.abs(rel)
    me = 8
    nf = np.maximum(n, 1).astype(np.float32)
    large = me + (np.log(nf / np.float32(me)) / np.float32(np.log(128 / me)) * np.float32(nb - me)).astype(np.int32)
    large = np.minimum(large, nb - 1)
    return ret + np.where(n < me, n, large)


def host_consts(inp):
    f32 = np.float32
    c = {}
    c["ident"] = np.eye(128, dtype=f32)
    pc = np.arange(128) // 64
    c["vis"] = np.where(pc[None, :] <= pc[:, None], 0.0, NEG).astype(f32)
    c["pw"] = np.broadcast_to((2.0 ** -np.arange(T_BIS + 1)).astype(f32), (128, T_BIS + 1)).copy()
    gcol = np.zeros((128, 16), f32)
    gcol[:, 0:8] = np.asarray(inp["g_pre_mix"], f32).reshape(8, 128).T
    gcol[:, 8:16] = np.asarray(inp["g_pre_ffn"], f32).reshape(8, 128).T
    c["gcol"] = gcol
    gpost = np.zeros((128, 2048), f32)
    gpost[:, 0:1024] = np.asarray(inp["g_post_mix"], f32).reshape(1, 1024)
    gpost[:, 1024:2048] = np.asarray(inp["g_post_ffn"], f32).reshape(1, 1024)
    c["gpost"] = gpost
    ln = np.zeros((128, 1024), f32)
    ln[:, 0:512] = np.asarray(inp["sgu_ln_g"], f32).reshape(1, 512)
    ln[:, 512:1024] = np.asarray(inp["sgu_ln_b"], f32).reshape(1, 512)
    c["lngb"] = ln
    rb = np.asarray(inp["rel_bias"], f32)
    p = np.arange(128)[:, None]
    j = np.arange(256)[None, :]
    bucket = _t5_bucket(j - 128 - p)
    bh = np.zeros((128, 4 * 256 + 4), f32)
    for h in range(4):
        bh[:, 256 * h:256 * h + 256] = rb[bucket, h]
        bh[:, 1024 + h] = rb[15, h]
    c["bh"] = bh
    sw = np.asarray(inp["sgu_w"], f32).reshape(4, 128, 128)
    c["wsT"] = np.ascontiguousarray(sw.transpose(2, 0, 1).reshape(128, 512))
    c["bsT"] = np.ascontiguousarray(np.asarray(inp["sgu_b"], f32).reshape(4, 128).T)
    return c


def make_in_maps(inputs, n_cores, S):
    c = host_consts(inputs)
    shared = dict(c)
    shared["w_in"] = np.ascontiguousarray(np.asarray(inputs["w_in"], np.float32).reshape(D_MODEL, 3144))
    shared["w_o"] = np.ascontiguousarray(np.asarray(inputs["w_o"], np.float32).reshape(D_MODEL, D_MODEL))
    shared["w_gate"] = np.ascontiguousarray(np.asarray(inputs["w_gate"], np.float32).reshape(D_MODEL, D_FF))
    shared["w_up"] = np.ascontiguousarray(np.asarray(inputs["w_up"], np.float32).reshape(D_MODEL, D_FF))
    shared["w_down"] = np.ascontiguousarray(np.asarray(inputs["w_down"], np.float32).reshape(D_FF, D_MODEL))
    x = np.asarray(inputs["x"], np.float32)
    maps = []
    for b in range(n_cores):
        m = dict(shared)
        m["x"] = np.ascontiguousarray(x[b, :S])
        maps.append(m)
    return maps


def kernel(**inputs):
    n = 8
    nc = build(NB=32, debug=False)
    in_maps = make_in_maps(inputs, n, SEQ)
    res = run_bass_kernel_spmd(nc, in_maps, core_ids=list(range(n)))
    out = np.stack([np.asarray(r["y"], np.float32) for r in res.results], axis=0)
    return out
```

```python
import numpy as np
from contextlib import ExitStack
import concourse.bass as bass
import concourse.mybir as mybir
from concourse.bass_utils import run_bass_kernel_spmd
from concourse.alu_op_type import AluOpType as ALU

F32 = mybir.dt.float32
BF16 = mybir.dt.bfloat16
AF = mybir.ActivationFunctionType
AX = mybir.AxisListType

D_MODEL = 1024
SEQ = 4096
D_FF = 2816
NFF = D_FF // 128
TOPK = 256
T_BIS = 14
EPS = 1e-6
NEG = -1.0e30
QSCALE = 128 ** -0.5
ISCALE = (64 ** -0.5) * (8 ** -0.5)


class _Stop(Exception):
    pass


class Sched:
    def __init__(self, nc, es):
        self.nc = nc
        self.es = es
        self.eng = {"pe": nc.tensor, "act": nc.scalar, "dve": nc.vector, "pool": nc.gpsimd, "sp": nc.sync}
        self.semh = {}
        self.cnt = {}
        for e in self.eng:
            self.semh[e] = es.enter_context(nc.semaphore("sem_" + e))
            self.cnt[e] = 0
        self.seen = {e: {} for e in self.eng}
        self.lastw = {}
        self.readers = {}
        self.nwait = 0
        self.nops = 0
        self.stop_ops = None
        self.log = None
        self.rec = None

    def _dma_key(self, slot):
        key = "dma:" + slot
        if key not in self.semh:
            self.semh[key] = self.es.enter_context(self.nc.semaphore("sd_" + slot))
            self.cnt[key] = 0
        return key

    PSUM_RES = frozenset(["MM0", "MM1", "MM2", "S0", "OB0", "OB1", "T0", "T1"])

    def record(self, f):
        saved = self.rec
        self.rec = []
        f()
        out = self.rec
        self.rec = saved
        return out

    def interleave(self, la, lb):
        na, nb = len(la), len(lb)
        ia = ib = 0
        import os
        if os.environ.get("NOINTER"):
            for a in la: self.op(*a)
            for b in lb: self.op(*b)
            return
        while ia < na or ib < nb:
            if ib >= nb or (ia < na and ia * nb <= ib * na):
                self.op(*la[ia])
                ia += 1
            else:
                self.op(*lb[ib])
                ib += 1

    def op(self, eng, fn, reads=(), writes=(), dma=None):
        if self.rec is not None:
            self.rec.append((eng, fn, tuple(reads), tuple(writes), dma))
            return None
        pr = [r for r in reads if r in self.PSUM_RES]
        if pr:
            writes = list(writes) + pr
        deps = {}

        def add(d, raw):
            if d is None:
                return
            key, val, deng = d
            if deng == eng and not key.startswith("dma:"):
                if eng in ("pe", "sp"):
                    return
            if deps.get(key, 0) < val:
                deps[key] = val

        for r in reads:
            add(self.lastw.get(r), True)
        for w in writes:
            add(self.lastw.get(w), False)
            for key, (val, deng) in self.readers.get(w, {}).items():
                add((key, val, deng), False)
        E = self.eng[eng]
        for key, val in deps.items():
            if self.seen[eng].get(key, 0) < val:
                E.wait_ge(self.semh[key], val)
                self.seen[eng][key] = val
                self.nwait += 1
        if self.stop_ops is not None and self.nops >= self.stop_ops:
            raise _Stop()
        inst = fn(E)
        if self.log is not None:
            import sys as _s
            self.log.append((self.nops, eng, _s._getframe(1).f_lineno, tuple(reads), tuple(writes)))
        self.nops += 1
        if dma is not None:
            key = self._dma_key(dma)
            self.cnt[key] += 16
            inst.then_inc(self.semh[key], 16)
        else:
            key = eng
            self.cnt[key] += 1
            inst.then_inc(self.semh[key], 1)
        val = self.cnt[key]
        for r in reads:
            d = self.readers.setdefault(r, {})
            if d.get(key, (0, None))[0] < val:
                d[key] = (val, eng)
        for w in writes:
            self.lastw[w] = (key, val, eng)
            self.readers[w] = {}
        return inst

    def barrier(self):
        for e in self.eng:
            E = self.eng[e]
            for key, val in self.cnt.items():
                if val > 0 and key != e and self.seen[e].get(key, 0) < val:
                    E.wait_ge(self.semh[key], val)
                    self.seen[e][key] = val

    def final_wait(self, eng="sp"):
        E = self.eng[eng]
        for key, val in self.cnt.items():
            if val > 0 and key != eng and self.seen[eng].get(key, 0) < val:
                E.wait_ge(self.semh[key], val)
                self.seen[eng][key] = val


def skew_emit(items, nstage):
    n = len(items)
    for step in range(n + nstage - 1):
        for s in range(nstage):
            j = step - s
            if 0 <= j < n:
                items[j][s]()


def build(NB=32, debug=False, stop=None, stop_ops=None):
    nc = bass.Bass("TRN2", target_bir_lowering=False)
    es = ExitStack()
    sc = Sched(nc, es)
    sc.stop_ops = stop_ops
    try:
        _build_body(nc, es, sc, NB, debug, stop)
    except _Stop:
        pass
    sc.final_wait("sp")
    es.close()
    nc._sched_stats = (sc.nops, sc.nwait)
    return nc


def _build_body(nc, es, sc, NB, debug, stop):
    def check(tag):
        if stop == tag:
            raise _Stop()

    S = NB * 128
    NG = NB // 4
    dt = nc.dram_tensor
    x_d = dt("x", [S, D_MODEL], F32, kind="ExternalInput").ap()
    w_in_d = dt("w_in", [D_MODEL, 3144], F32, kind="ExternalInput").ap()
    w_o_d = dt("w_o", [D_MODEL, D_MODEL], F32, kind="ExternalInput").ap()
    w_g_d = dt("w_gate", [D_MODEL, D_FF], F32, kind="ExternalInput").ap()
    w_u_d = dt("w_up", [D_MODEL, D_FF], F32, kind="ExternalInput").ap()
    w_d_d = dt("w_down", [D_FF, D_MODEL], F32, kind="ExternalInput").ap()
    ident_d = dt("ident", [128, 128], F32, kind="ExternalInput").ap()
    vis_d = dt("vis", [128, 128], F32, kind="ExternalInput").ap()
    pw_d = dt("pw", [128, T_BIS + 1], F32, kind="ExternalInput").ap()
    gcol_d = dt("gcol", [128, 16], F32, kind="ExternalInput").ap()
    gpost_d = dt("gpost", [128, 2048], F32, kind="ExternalInput").ap()
    ln_d = dt("lngb", [128, 1024], F32, kind="ExternalInput").ap()
    bh_d = dt("bh", [128, 4 * 256 + 4], F32, kind="ExternalInput").ap()
    wsT_d = dt("wsT", [128, 512], F32, kind="ExternalInput").ap()
    bs_d = dt("bsT", [128, 4], F32, kind="ExternalInput").ap()
    y_d = dt("y", [S, D_MODEL], F32, kind="ExternalOutput").ap()
    if debug:
        dbg_ob = dt("dbg_ob", [S, 512], BF16, kind="ExternalOutput").ap()
        dbg_sc = dt("dbg_sc", [128, 4096], F32, kind="ExternalOutput").ap()
        dbg_thr = dt("dbg_thr", [128, NB], F32, kind="ExternalOutput").ap()
        dbg_x1 = dt("dbg_x1", [S, D_MODEL], F32, kind="ExternalOutput").ap()

    op = sc.op

    def sb(name, shape, dtype, stack=es):
        return stack.enter_context(nc.sbuf_tensor("s_" + name, shape, dtype))

    def ps(name, shape, dtype, stack=es):
        return stack.enter_context(nc.psum_tensor("p_" + name, shape, dtype))

    ARENA_BYTES = 198 * 1024
    arena = es.enter_context(nc.sbuf_tensor("s_arena", [128, ARENA_BYTES // 2], BF16))
    apos = [0]

    def aset(off_bytes):
        apos[0] = off_bytes

    def av(name, shape, dtype, stack=None):
        esz = 4 if dtype == F32 else 2
        nel = 1
        for d in shape[1:]:
            nel *= d
        nbytes = (nel * esz + 63) // 64 * 64
        off = apos[0]
        assert off % 64 == 0 and off + nbytes <= ARENA_BYTES, (name, off, nbytes)
        apos[0] = off + nbytes
        v = arena[:, off // 2:off // 2 + nel * esz // 2]
        if dtype == F32:
            v = v.bitcast(F32)
        if len(shape) == 3:
            v = v.rearrange("p (a b) -> p a b", a=shape[1])
        elif len(shape) == 4:
            v = v.rearrange("p (a b c) -> p a b c", a=shape[1], b=shape[2])
        return v

    T0 = ps("T0", [128, 1024], BF16)
    T1 = ps("T1", [128, 1024], BF16)
    MM = [ps("MM%d" % i, [128, 512], F32) for i in range(3)]
    S0 = ps("S0", [128, 512], F32)
    OB = [ps("OB%d" % i, [128, 512], F32) for i in range(2)]
    TB = [T0, T1]
    LGB = [(MM[2][:, :], "MM2"), (T1[:, :].bitcast(F32), "T1")]

    ident = sb("ident", [128, 128], BF16)
    gcol = sb("gcol", [128, 16], F32)
    junk = sb("junk", [128, 1024], BF16)
    ss = sb("ss", [128, 8], F32)
    rstd = sb("rstd", [128, 8], F32)
    hb = sb("hb", [128, 1024], BF16)
    epsb = sb("epsb", [128, 2], F32)
    aset(0)
    outb = av("outb", [128, 32, 512], BF16)
    xs = av("xs", [128, 1024], F32)
    hT = av("hT", [128, 1024], BF16)
    P1A_BASE = 38 * 1024
    aset(P1A_BASE)
    sb = av
    p1a = None
    vis = sb("vis", [128, 128], F32, p1a)
    pw = sb("pw", [128, T_BIS + 1], F32, p1a)
    bh = sb("bh", [128, 4 * 256 + 4], F32, p1a)

    op("pool", lambda e: e.dma_start(out=ident[:], in_=ident_d[:, :]), writes=["ident"], dma="c_ident")
    op("sp", lambda e: e.dma_start(out=vis[:], in_=vis_d[:, :]), writes=["vis"], dma="c0")
    op("sp", lambda e: e.dma_start(out=pw[:], in_=pw_d[:, :]), writes=["pw"], dma="c1")
    op("sp", lambda e: e.dma_start(out=gcol[:], in_=gcol_d[:, :]), writes=["gcol"], dma="c2")
    op("sp", lambda e: e.dma_start(out=bh[:], in_=bh_d[:, :]), writes=["bh"], dma="c4")

    def rms_rstd(src_ops, col):
        op("act", lambda e: e.activation(out=rstd[:, col:col + 1], in_=ss[:, col:col + 1], func=AF.Ln,
                                         bias=epsb[:, 0:1], scale=1.0 / 1024),
           reads=["ss%d" % col, "epsb"], writes=["rstd%d" % col])
        op("act", lambda e: e.activation(out=rstd[:, col:col + 1], in_=rstd[:, col:col + 1], func=AF.Exp,
                                         scale=-0.5),
           reads=["rstd%d" % col], writes=["rstd%d" % col])

    op("dve", lambda e: e.memset(epsb[:, 0:1], EPS), writes=["epsb"])
    check("consts")

    def load_norm_transpose(i, gc0):
        t0 = i * 128
        op("sp", lambda e: e.dma_start(out=xs[:], in_=x_d[t0:t0 + 128, :]), writes=["xs"], dma="xs")
        op("act", lambda e: e.activation(out=junk[:, 0:1024], in_=xs[:], func=AF.Square, accum_out=ss[:, 0:1]),
           reads=["xs"], writes=["ss0", "junk"])
        rms_rstd(None, 0)
        op("dve", lambda e: e.tensor_scalar(out=hb[:], in0=xs[:], scalar1=rstd[:, 0:1], scalar2=None, op0=ALU.mult),
           reads=["xs", "rstd0"], writes=["hb"])
        for k in range(8):
            op("pe", lambda e, k=k: e.transpose(out=T0[:, 128 * k:128 * k + 128], in_=hb[:, 128 * k:128 * k + 128],
                                                identity=ident[:]),
               reads=["hb", "ident"], writes=["T0"])
        for k in range(8):
            if True:
                op("dve", lambda e, k=k: e.tensor_scalar(out=hT[:, 128 * k:128 * k + 128], in0=T0[:, 128 * k:128 * k + 128],
                                                         scalar1=gcol[:, gc0 + k:gc0 + k + 1], scalar2=None, op0=ALU.mult),
                   reads=["T0", "gcol"], writes=["hT%d" % k])
            else:
                op("act", lambda e, k=k: e.activation(out=hT[:, 128 * k:128 * k + 128], in_=T0[:, 128 * k:128 * k + 128],
                                                      func=AF.Identity, scale=gcol[:, gc0 + k:gc0 + k + 1]),
                   reads=["T0", "gcol"], writes=["hT%d" % k])

    w1 = sb("w1", [128, 8, 2120], BF16, p1a)
    kT = sb("kT", [128, 4, S], BF16, p1a)
    vc = sb("vc", [128, NB, 4, 130], BF16, p1a)
    ikT = sb("ikT", [128, S], BF16, p1a)
    score = sb("score", [128, S], F32, p1a)
    sel = sb("sel", [128, S], BF16, p1a)
    q_sb = sb("q_sb", [128, 512], BF16, p1a)
    k_sb = sb("k_sb", [128, 512], BF16, p1a)
    iq_sb = sb("iq_sb", [128, 512], BF16, p1a)
    ik2 = sb("ik2", [128, 128], BF16, p1a)
    qT2 = [sb("qT%d" % j, [128, 512], BF16, p1a) for j in range(2)]
    iqT2 = [sb("iqT%d" % j, [128, 512], BF16, p1a) for j in range(2)]
    Dg2 = [sb("Dg%d" % j, [128, 8, 128], BF16, p1a) for j in range(2)]
    iwc2 = [sb("iwc%d" % j, [128, 8], F32, p1a) for j in range(2)]
    junkd = sb("junkd", [128, 512], BF16, p1a)
    rbuf = [sb("r%d" % j, [128, 512], BF16, p1a) for j in range(3)]
    Eb = [sb("E%d" % j, [128, 512], BF16, p1a) for j in range(2)]
    Pb = [sb("P%d" % j, [128, 512], BF16, p1a) for j in range(2)]
    PTb = [sb("PT%d" % j, [128, 512], BF16, p1a) for j in range(2)]
    tmpb = sb("tmpb", [128, 256], F32, p1a)
    amaxp = sb("amaxp", [128, 8], F32, p1a)
    aminp = sb("aminp", [128, 8], F32, p1a)
    bis = sb("bis", [128, 8], F32, p1a)
    D2 = sb("D2", [128, T_BIS + 1], F32, p1a)
    thr_all = sb("thr_all", [128, NB], F32, p1a)

    for k in range(8):
        op("pool", lambda e, k=k: e.dma_start(out=w1[:, k, :], in_=w_in_d[128 * k:128 * k + 128, 1024:3144],
                                              max_dma_last_dim=4096),
           writes=["w1_%d" % k], dma="w1_%d" % k)
    op("dve", lambda e: e.memset(vc[:, :, :, 128:130], 1.0), writes=["vc_ones"])

    dots_rot = [0]
    lg_rot = [0]
    e_rot = [0]
    tb_rot = [0]

    def p1a_block(i, st):
        t0 = i * 128
        L = t0 + 128
        par = i % 2
        nkt = (L + 511) // 512
        qT, iqT, Dg, iwc = qT2[par], iqT2[par], Dg2[par], iwc2[par]
        nqT, niqT, niwc = "qT_%d" % par, "iqT_%d" % par, "iwc_%d" % par
        if st == "A":
            load_norm_transpose(i, 0)
            groups = [("q", 0, 512), ("k", 512, 512), ("v", 1024, 512), ("iq", 1536, 512), ("ik", 2048, 72)]
            for gi, (nm, c0, w) in enumerate(groups):
                bank = MM[gi % 3]
                bname = "MM%d" % (gi % 3)
                for k in range(8):
                    op("pe", lambda e, k=k, bank=bank, c0=c0, w=w: e.matmul(
                        bank[:, 0:w], lhsT=hT[:, 128 * k:128 * k + 128], rhs=w1[:, k, c0:c0 + w],
                        start=(k == 0), stop=(k == 7)),
                       reads=["hT%d" % k, "w1_%d" % k], writes=[bname])
                if nm == "q":
                    op("act", lambda e, bank=bank: e.activation(out=q_sb[:], in_=bank[:, :], func=AF.Copy),
                       reads=[bname], writes=["q_sb"])
                elif nm == "k":
                    op("dve", lambda e, bank=bank: e.tensor_copy(out=k_sb[:], in_=bank[:, :]),
                       reads=[bname], writes=["k_sb"])
                elif nm == "v":
                    op("act", lambda e, bank=bank, i=i: e.activation(
                        out=vc[:, i, :, 0:128], in_=bank[:, :].rearrange("p (h d) -> p h d", h=4), func=AF.Copy),
                       reads=[bname], writes=["vc:%d" % i])
                elif nm == "iq":
                    op("dve", lambda e, bank=bank: e.tensor_copy(out=iq_sb[:], in_=bank[:, :]),
                       reads=[bname], writes=["iq_sb"])
                else:
                    op("act", lambda e, bank=bank: e.activation(out=ik2[:, 0:64], in_=bank[:, 0:64], func=AF.Copy),
                       reads=[bname], writes=["ik2a"])
                    op("dve", lambda e, bank=bank: e.tensor_copy(out=ik2[:, 64:128], in_=bank[:, 0:64]),
                       reads=[bname], writes=["ik2b"])
                    op("dve", lambda e, bank=bank: e.tensor_scalar(out=iwc[:], in0=bank[:, 64:72], scalar1=ISCALE,
                                                                   scalar2=None, op0=ALU.mult),
                       reads=[bname], writes=[niwc])
            for h in range(4):
                op("pe", lambda e, h=h: e.transpose(out=T1[:, 128 * h:128 * h + 128], in_=q_sb[:, 128 * h:128 * h + 128],
                                                    identity=ident[:]),
                   reads=["q_sb", "ident"], writes=["T1"])
            for h in range(4):
                op("pe", lambda e, h=h: e.transpose(out=T1[:, 512 + 128 * h:512 + 128 * h + 128],
                                                    in_=k_sb[:, 128 * h:128 * h + 128], identity=ident[:]),
                   reads=["k_sb", "ident"], writes=["T1"])
            op("dve", lambda e: e.tensor_copy(out=qT[:], in_=T1[:, 0:512]), reads=["T1"], writes=[nqT])
            op("dve", lambda e, t0=t0: e.tensor_copy(out=kT[:, :, t0:t0 + 128],
                                                     in_=T1[:, 512:1024].rearrange("p (h t) -> p h t", h=4)),
               reads=["T1"], writes=["kT:%d" % i])
            for h in range(4):
                op("pe", lambda e, h=h: e.transpose(out=T0[:, 128 * h:128 * h + 128], in_=iq_sb[:, 128 * h:128 * h + 128],
                                                    identity=ident[:]),
                   reads=["iq_sb", "ident"], writes=["T0"])
            op("pe", lambda e: e.transpose(out=T0[:, 512:640], in_=ik2[:, :], identity=ident[:]),
               reads=["ik2a", "ik2b", "ident"], writes=["T0"])
            op("dve", lambda e: e.tensor_copy(out=iqT[:], in_=T0[:, 0:512]), reads=["T0"], writes=[niqT])
            op("dve", lambda e, t0=t0: e.tensor_copy(out=ikT[:, t0:t0 + 128], in_=T0[:, 512:640]),
               reads=["T0"], writes=["ikT:%d" % i])
            for h in range(8):
                op("dve", lambda e, h=h: e.tensor_scalar(out=Dg[:, h, :], in0=ident[:], scalar1=iwc[:, h:h + 1],
                                                          scalar2=1.0, op0=ALU.mult, op1=ALU.mult),
                   reads=["ident", niwc], writes=["Dg%d_%d" % (par, h)])

        if st == "I":
            pairs = [(kt, h) for kt in range(nkt) for h in range(8)]

            def emit_dots(n):
                kt, h = pairs[n]
                N = min(512, L - 512 * kt)
                b = dots_rot[0] % 2
                dots_rot[0] += 1
                pb = 64 * (h % 2)
                cb = 128 * (h // 2)
                blks = ["ikT:%d" % bb for bb in range(4 * kt, min(4 * kt + 4, i + 1))]
                op("pe", lambda e: e.matmul(MM[b][:, 0:N], lhsT=iqT[pb:pb + 64, cb:cb + 128],
                                            rhs=ikT[pb:pb + 64, 512 * kt:512 * kt + N], start=True, stop=True),
                   reads=[niqT] + blks, writes=["MM%d" % b])
                return b

            dbank = {}
            for n in range(min(2, len(pairs))):
                dbank[n] = emit_dots(n)
            for n, (kt, h) in enumerate(pairs):
                N = min(512, L - 512 * kt)
                b = dbank[n]
                rj = n % 3
                if h % 2 == 0:
                    op("act", lambda e, b=b, rj=rj, N=N: e.activation(out=rbuf[rj][:, 0:N], in_=MM[b][:, 0:N], func=AF.Relu),
                       reads=["MM%d" % b], writes=["r%d" % rj])
                else:
                    op("dve", lambda e, b=b, rj=rj, N=N: e.tensor_scalar(out=rbuf[rj][:, 0:N], in0=MM[b][:, 0:N], scalar1=0.0,
                                                                         scalar2=None, op0=ALU.max),
                       reads=["MM%d" % b], writes=["r%d" % rj])
                op("pe", lambda e, rj=rj, N=N, h=h: e.matmul(S0[:, 0:N], lhsT=Dg[:, h, :], rhs=rbuf[rj][:, 0:N],
                                                             start=(h == 0), stop=(h == 7)),
                   reads=["r%d" % rj, "Dg%d_%d" % (par, h)], writes=["S0"])
                if n + 2 < len(pairs):
                    dbank[n + 2] = emit_dots(n + 2)
                if h == 7:
                    op("act", lambda e, kt=kt, N=N: e.activation(out=score[:, 512 * kt:512 * kt + N], in_=S0[:, 0:N],
                                                                 func=AF.Copy),
                       reads=["S0"], writes=["score"])
                    op("dve", lambda e, kt=kt, N=N: e.tensor_scalar(
                        out=junkd[:, 0:N], in0=score[:, 512 * kt:512 * kt + N], scalar1=-3.0e38, scalar2=None,
                        op0=ALU.max, op1=ALU.max, accum_out=amaxp[:, kt:kt + 1]),
                       reads=["score"], writes=["amaxp", "junkd"])
                    op("dve", lambda e, kt=kt, N=N: e.tensor_scalar(
                        out=junkd[:, 0:N], in0=score[:, 512 * kt:512 * kt + N], scalar1=3.0e38, scalar2=None,
                        op0=ALU.min, op1=ALU.min, accum_out=aminp[:, kt:kt + 1]),
                       reads=["score"], writes=["aminp", "junkd"])
            op("dve", lambda e, L=L: e.tensor_tensor(out=score[:, L - 128:L], in0=score[:, L - 128:L], in1=vis[:],
                                                      op=ALU.add),
               reads=["score", "vis"], writes=["score"])

        if st == "B":
            A_, MID, CNT, PM, THR = (bis[:, j:j + 1] for j in range(5))
            if L > TOPK:
                op("dve", lambda e: e.tensor_scalar(out=amaxp[:, 0:nkt], in0=amaxp[:, 0:nkt], scalar1=-3.0e38, scalar2=None,
                                                    op0=ALU.max, op1=ALU.max, accum_out=bis[:, 6:7]),
                   reads=["amaxp"], writes=["amaxp", "bis6"])
                op("dve", lambda e: e.tensor_scalar(out=aminp[:, 0:nkt], in0=aminp[:, 0:nkt], scalar1=3.0e38, scalar2=None,
                                                    op0=ALU.min, op1=ALU.min, accum_out=bis[:, 7:8]),
                   reads=["aminp"], writes=["aminp", "bis7"])
                op("dve", lambda e: e.scalar_tensor_tensor(out=A_, in0=bis[:, 7:8], scalar=-1.0, in1=bis[:, 6:7],
                                                           op0=ALU.mult, op1=ALU.max),
                   reads=["bis6", "bis7"], writes=["bisA"])
                op("dve", lambda e: e.tensor_scalar(out=D2[:], in0=pw[:], scalar1=A_, scalar2=None, op0=ALU.mult),
                   reads=["bisA", "pw"], writes=["D2"])
                op("dve", lambda e: e.memset(MID, 0.0), writes=["mid"])
                Ld = L if L < 1024 else ((L * 9 // 20 + 127) // 128) * 128
                nA = L - Ld
                for k in range(T_BIS):
                    op("dve", lambda e: e.tensor_scalar(out=sel[:, 0:Ld], in0=score[:, 0:Ld], scalar1=MID, scalar2=None,
                                                        op0=ALU.is_gt, op1=ALU.add, accum_out=CNT),
                       reads=["score", "mid"], writes=["cnt", "sel"])
                    if nA > 0:
                        op("act", lambda e: e.activation(out=sel[:, Ld:L], in_=score[:, Ld:L], func=AF.Sign,
                                                         bias=MID, scale=-1.0, accum_out=bis[:, 6:7]),
                           reads=["score", "mid"], writes=["bis6", "selB"])
                        op("dve", lambda e: e.scalar_tensor_tensor(out=CNT, in0=bis[:, 6:7], scalar=-0.5, in1=CNT,
                                                                   op0=ALU.mult, op1=ALU.add),
                           reads=["bis6", "cnt"], writes=["cnt"])
                    op("dve", lambda e: e.tensor_scalar(out=PM, in0=CNT, scalar1=TOPK - 0.5 - nA / 2.0, scalar2=0.5,
                                                        op0=ALU.is_gt, op1=ALU.subtract),
                       reads=["cnt"], writes=["pm"])
                    op("dve", lambda e, k=k: e.scalar_tensor_tensor(out=MID, in0=PM, scalar=D2[:, k:k + 1], in1=MID,
                                                                    op0=ALU.mult, op1=ALU.add),
                       reads=["pm", "D2", "mid"], writes=["mid"])
                op("dve", lambda e: e.tensor_tensor(out=THR, in0=MID, in1=D2[:, T_BIS:T_BIS + 1], op=ALU.subtract),
                   reads=["mid", "D2"], writes=["thr"])
            else:
                op("dve", lambda e: e.memset(THR, -1.0e29), writes=["thr"])
            if debug:
                op("dve", lambda e, i=i: e.tensor_copy(out=thr_all[:, i:i + 1], in_=THR), reads=["thr"], writes=["thr_all"])
            op("dve", lambda e, L=L: e.tensor_scalar(out=sel[:, 0:L], in0=score[:, 0:L], scalar1=THR, scalar2=None,
                                                     op0=ALU.is_gt),
               reads=["score", "thr"], writes=["sel", "selB"])

        if st == "C":
            ntile = (L + 511) // 512
            bw = min(256, L)
            items = []
            for h in range(4):
                for j in range(ntile - 1, -1, -1):
                    c1 = L - 512 * j
                    c0 = max(0, c1 - 512)
                    N = c1 - c0
                    first = (j == ntile - 1)
                    last = (j == 0)
                    st = {}

                    def s1(h=h, c0=c0, c1=c1, N=N, j=j, st=st):
                        LG, lgn = LGB[lg_rot[0] % 2]
                        lg_rot[0] += 1
                        ej = e_rot[0] % 2
                        e_rot[0] += 1
                        st["ej"] = ej
                        blks = ["kT:%d" % bb for bb in range(c0 // 128, c1 // 128)]
                        op("pe", lambda e: e.matmul(LG[:, 0:N], lhsT=qT[:, 128 * h:128 * h + 128], rhs=kT[:, h, c0:c1],
                                                    start=True, stop=True),
                           reads=[nqT] + blks, writes=[lgn])
                        nb_ = bw if j == 0 else 0
                        nf = N - nb_
                        if nf > 0:
                            op("act", lambda e: e.activation(out=Eb[ej][:, 0:nf], in_=LG[:, 0:nf], func=AF.Exp,
                                                             bias=bh[:, 1024 + h:1025 + h], scale=QSCALE),
                               reads=[lgn, "bh"], writes=["E%d" % ej])
                        if nb_ > 0:
                            op("dve", lambda e: e.scalar_tensor_tensor(
                                out=tmpb[:, 0:nb_], in0=LG[:, nf:N], scalar=QSCALE,
                                in1=bh[:, 256 * h + 256 - nb_:256 * h + 256], op0=ALU.mult, op1=ALU.add),
                               reads=[lgn, "bh"], writes=["tmpb"])
                            op("act", lambda e: e.activation(out=Eb[ej][:, nf:N], in_=tmpb[:, 0:nb_], func=AF.Exp),
                               reads=["tmpb"], writes=["E%d" % ej])
                        op("dve", lambda e: e.tensor_tensor(out=Pb[ej][:, 0:N], in0=Eb[ej][:, 0:N], in1=sel[:, c0:c1],
                                                            op=ALU.mult),
                           reads=["E%d" % ej, "sel", "selB"], writes=["P%d" % ej])

                    def s2(N=N, st=st):
                        ej = st["ej"]
                        tb = 0
                        pj = tb_rot[0] % 2
                        tb_rot[0] += 1
                        st["pj"] = pj
                        st["tb"] = tb
                        for m in range(N // 128):
                            op("pe", lambda e, m=m: e.transpose(out=TB[tb][:, 128 * m:128 * m + 128],
                                                                in_=Pb[ej][:, 128 * m:128 * m + 128], identity=ident[:]),
                               reads=["P%d" % ej, "ident"], writes=["T%d" % tb])
                        eng = "dve"
                        if eng == "act":
                            op("act", lambda e: e.activation(out=PTb[pj][:, 0:N], in_=TB[tb][:, 0:N], func=AF.Copy),
                               reads=["T%d" % tb], writes=["PT%d" % pj])
                        else:
                            op("dve", lambda e: e.tensor_copy(out=PTb[pj][:, 0:N], in_=TB[tb][:, 0:N]),
                               reads=["T%d" % tb], writes=["PT%d" % pj])

                    def s3(h=h, c0=c0, N=N, first=first, last=last, st=st, i=i):
                        pj = st["pj"]
                        ob = OB[h // 2]
                        off = 130 * (h % 2)
                        nm = N // 128
                        for m in range(nm):
                            blk = c0 // 128 + m
                            op("pe", lambda e, m=m, blk=blk: e.matmul(
                                ob[:, off:off + 130], lhsT=PTb[pj][:, 128 * m:128 * m + 128], rhs=vc[:, blk, h, :],
                                start=(first and m == 0), stop=(last and m == nm - 1)),
                               reads=["PT%d" % pj, "vc:%d" % blk, "vc_ones"], writes=["OB%d" % (h // 2)])
                        if last:
                            RS = bis[:, 5:6]
                            op("dve", lambda e: e.reciprocal(out=RS, in_=ob[:, off + 128:off + 129]),
                               reads=["OB%d" % (h // 2)], writes=["rs"])
                            op("dve", lambda e: e.tensor_scalar(out=outb[:, i, 128 * h:128 * h + 128], in0=ob[:, off:off + 128],
                                                                scalar1=RS, scalar2=None, op0=ALU.mult),
                               reads=["OB%d" % (h // 2), "rs"], writes=["outb:%d" % i])

                    items.append([s1, s2, s3])
            skew_emit(items, 3)

    rec = sc.record
    p1a_block(0, 'A')
    p1a_block(0, 'I')
    for i in range(NB):
        la = rec(lambda: p1a_block(i, 'B'))
        lb = rec(lambda: p1a_block(i + 1, 'A')) if i + 1 < NB else []
        sc.interleave(la, lb)
        la = rec(lambda: p1a_block(i, 'C'))
        lb = rec(lambda: p1a_block(i + 1, 'I')) if i + 1 < NB else []
        sc.interleave(la, lb)
        check("C%d" % i)

    if debug:
        op("sp", lambda e: e.dma_start(out=dbg_sc[:, 0:S], in_=score[:, 0:S]), reads=["score"], dma="dbg0")
        op("sp", lambda e: e.dma_start(out=dbg_thr[:, :], in_=thr_all[:]), reads=["thr_all"], dma="dbg1")
        for i in range(NB):
            op("sp", lambda e, i=i: e.dma_start(out=dbg_ob[128 * i:128 * i + 128, :], in_=outb[:, i, :]),
               reads=["outb:%d" % i], dma="dbg2")
    sc.barrier()

    check("P1a")
    aset(P1A_BASE)
    wg = sb("wg", [128, 8, D_FF], BF16)
    wu = sb("wu", [128, 8, D_FF], BF16)
    P1B_BASE = apos[0]
    p1b = None
    w1b = sb("w1b", [128, 8, 1024], BF16, p1b)
    wo = sb("wo", [128, 8, 1024], BF16, p1b)
    lngb = sb("lngb", [128, 1024], F32, p1b)
    wsT = sb("wsT", [128, 512], BF16, p1b)
    bsT = sb("bsT", [128, 4], F32, p1b)
    u_sb = sb("u_sb", [128, 512], F32, p1b)
    avb = sb("av", [128, 512], F32, p1b)
    vn = sb("vn", [128, 512], BF16, p1b)
    cata = sb("cata", [128, 512], BF16, p1b)
    catT = sb("catT", [128, 1024], BF16, p1b)
    tmpx = sb("tmpx", [128, 1024], F32, p1b)
    bst = sb("bst", [128, 8], F32, p1b)
    mv = sb("mv", [128, 8], F32, p1b)
    gpost = sb("gpost1", [128, 1024], F32, p1b)
    op("sp", lambda e: e.dma_start(out=gpost[:], in_=gpost_d[:, 0:1024]), writes=["gpost"], dma="c3")

    for k in range(8):
        op("pool", lambda e, k=k: e.dma_start(out=w1b[:, k, :], in_=w_in_d[128 * k:128 * k + 128, 0:1024]),
           writes=["w1b_%d" % k], dma="w1b_%d" % k)
    for k in range(8):
        op("pool", lambda e, k=k: e.dma_start(out=wo[:, k, :], in_=w_o_d[128 * k:128 * k + 128, :]),
           writes=["wo_%d" % k], dma="wo_%d" % k)
    op("sp", lambda e: e.dma_start(out=lngb[:], in_=ln_d[:, :]), writes=["lngb"], dma="c0")
    op("pool", lambda e: e.dma_start(out=wsT[:], in_=wsT_d[:, :]), writes=["wsT"], dma="c_ident")
    op("sp", lambda e: e.dma_start(out=bsT[:], in_=bs_d[:, :]), writes=["bsT"], dma="c1")
    for g in range(4):
        op("dve", lambda e, g=g: e.memset(wsT[64:128, 128 * g:128 * g + 64], 0.0), reads=["wsT"], writes=["wsT"])

    ffn_loads = []
    for k in range(8):
        for half in range(2):
            c0 = half * 1408
            ffn_loads.append(lambda k=k, c0=c0, half=half: op(
                "pool", lambda e: e.dma_start(out=wg[:, k, c0:c0 + 1408], in_=w_g_d[128 * k:128 * k + 128, c0:c0 + 1408]),
                writes=["wg_%d_%d" % (k, half)], dma="wg_%d_%d" % (k, c0)))
            ffn_loads.append(lambda k=k, c0=c0, half=half: op(
                "pool", lambda e: e.dma_start(out=wu[:, k, c0:c0 + 1408], in_=w_u_d[128 * k:128 * k + 128, c0:c0 + 1408]),
                writes=["wu_%d_%d" % (k, half)], dma="wu_%d_%d" % (k, c0)))

    for i in range(NB):
        t0 = i * 128
        load_norm_transpose(i, 0)
        for gi in range(2):
            bank = MM[gi]
            for k in range(8):
                op("pe", lambda e, k=k, bank=bank, gi=gi: e.matmul(
                    bank[:, :], lhsT=hT[:, 128 * k:128 * k + 128], rhs=w1b[:, k, 512 * gi:512 * gi + 512],
                    start=(k == 0), stop=(k == 7)),
                   reads=["hT%d" % k, "w1b_%d" % k], writes=["MM%d" % gi])
        op("act", lambda e: e.activation(out=u_sb[:], in_=MM[0][:, :], func=AF.Copy), reads=["MM0"], writes=["u_sb"])
        op("dve", lambda e: e.tensor_copy(out=avb[:], in_=MM[1][:, :]), reads=["MM1"], writes=["av"])
        op("dve", lambda e: e.bn_stats(out=bst[:, 0:6], in_=avb[:]), reads=["av"], writes=["bst"])
        op("dve", lambda e: e.bn_aggr(out=mv[:, 0:2], in_=bst[:, 0:6]), reads=["bst"], writes=["mv"])
        op("act", lambda e: e.activation(out=mv[:, 2:3], in_=mv[:, 1:2], func=AF.Ln, bias=epsb[:, 0:1], scale=1.0),
           reads=["mv", "epsb"], writes=["mv2"])
        op("act", lambda e: e.activation(out=mv[:, 3:4], in_=mv[:, 2:3], func=AF.Exp, scale=-0.5),
           reads=["mv2"], writes=["mv3"])
        op("dve", lambda e: e.tensor_scalar(out=avb[:], in0=avb[:], scalar1=mv[:, 0:1], scalar2=mv[:, 3:4],
                                            op0=ALU.subtract, op1=ALU.mult),
           reads=["av", "mv", "mv3"], writes=["av"])
        op("dve", lambda e: e.tensor_tensor(out=avb[:], in0=avb[:], in1=lngb[:, 0:512], op=ALU.mult),
           reads=["av", "lngb"], writes=["av"])
        op("dve", lambda e: e.tensor_tensor(out=vn[:], in0=avb[:], in1=lngb[:, 512:1024], op=ALU.add),
           reads=["av", "lngb"], writes=["vn"])
        for g in range(4):
            op("pe", lambda e, g=g: e.matmul(MM[2][:, 128 * g:128 * g + 128], lhsT=wsT[:, 128 * g:128 * g + 128],
                                             rhs=vn[:, 128 * g:128 * g + 128], start=True, stop=True),
               reads=["wsT", "vn"], writes=["MM2"])
        for g in range(4):
            op("dve", lambda e, g=g: e.scalar_tensor_tensor(
                out=cata[:, 128 * g:128 * g + 128], in0=MM[2][:, 128 * g:128 * g + 128], scalar=bsT[:, g:g + 1],
                in1=u_sb[:, 128 * g:128 * g + 128], op0=ALU.add, op1=ALU.mult),
               reads=["MM2", "bsT", "u_sb"], writes=["cata"])
        for k in range(8):
            src = cata[:, 128 * k:128 * k + 128] if k < 4 else outb[:, i, 128 * (k - 4):128 * (k - 4) + 128]
            op("pe", lambda e, k=k, src=src: e.transpose(out=T1[:, 128 * k:128 * k + 128], in_=src, identity=ident[:]),
               reads=["cata", "outb:%d" % i, "ident"], writes=["T1"])
        op("dve", lambda e: e.tensor_copy(out=catT[:, 0:512], in_=T1[:, 0:512]), reads=["T1"], writes=["catTa"])
        op("dve", lambda e: e.tensor_copy(out=catT[:, 512:1024], in_=T1[:, 512:1024]), reads=["T1"], writes=["catTb"])
        for og in range(2):
            for k in range(8):
                op("pe", lambda e, k=k, og=og: e.matmul(
                    MM[og][:, :], lhsT=catT[:, 128 * k:128 * k + 128], rhs=wo[:, k, 512 * og:512 * og + 512],
                    start=(k == 0), stop=(k == 7)),
                   reads=["catTa" if k < 4 else "catTb", "wo_%d" % k], writes=["MM%d" % og])
        op("act", lambda e: e.activation(out=junk[:, 0:512], in_=MM[0][:, :], func=AF.Square, accum_out=ss[:, 2:3]),
           reads=["MM0"], writes=["ssa", "junk"])
        op("act", lambda e: e.activation(out=junk[:, 512:1024], in_=MM[1][:, :], func=AF.Square, accum_out=ss[:, 3:4]),
           reads=["MM1"], writes=["ssb", "junk"])
        op("dve", lambda e: e.tensor_tensor(out=ss[:, 1:2], in0=ss[:, 2:3], in1=ss[:, 3:4], op=ALU.add),
           reads=["ssa", "ssb"], writes=["ss1"])
        rms_rstd(None, 1)
        for og in range(2):
            op("dve", lambda e, og=og: e.scalar_tensor_tensor(
                out=tmpx[:, 512 * og:512 * og + 512], in0=MM[og][:, :], scalar=rstd[:, 1:2],
                in1=gpost[:, 512 * og:512 * og + 512], op0=ALU.mult, op1=ALU.mult),
               reads=["MM%d" % og, "rstd1", "gpost"], writes=["tmpx%d" % og])
        op("dve", lambda e: e.tensor_tensor(out=xs[:], in0=xs[:], in1=tmpx[:], op=ALU.add),
           reads=["xs", "tmpx0", "tmpx1"], writes=["xs"])
        op("sp", lambda e, t0=t0: e.dma_start(out=y_d[t0:t0 + 128, :], in_=xs[:]), reads=["xs"],
           writes=["y:%d" % i], dma="yst")
        if debug:
            op("sp", lambda e, t0=t0: e.dma_start(out=dbg_x1[t0:t0 + 128, :], in_=xs[:]), reads=["xs"], dma="dbg3")
        nper = (len(ffn_loads) + NB - 1) // NB
        for _ in range(nper):
            if ffn_loads:
                ffn_loads.pop(0)()
    while ffn_loads:
        ffn_loads.pop(0)()
    sc.barrier()

    check("P1b")
    p2 = None
    aset(P1B_BASE)
    wd = sb("wd", [128, NFF, D_MODEL], BF16, p2)
    aset(32 * 1024)
    gpost = sb("gpost2", [128, 1024], F32, p2)
    op("sp", lambda e: e.dma_start(out=gpost[:], in_=gpost_d[:, 1024:2048]), writes=["gpost"], dma="c3")
    aset(P1B_BASE + NFF * D_MODEL * 2)
    for c in range(NFF):
        op("pool", lambda e, c=c: e.dma_start(out=wd[:, c, :], in_=w_d_d[128 * c:128 * c + 128, :]),
           writes=["wd_%d" % c], dma="wd_%d" % c)
    actT = sb("actT", [128, NFF, 512], BF16, p2)
    aset(0)
    x1g = sb("x1g", [128, 4, 1024], F32, p2)
    h2T = sb("h2T", [128, 8, 512], BF16, p2)
    sg = [sb("sg%d" % j, [128, 512], F32, p2) for j in range(2)]
    outx = sb("outx", [128, 1024], F32, p2)
    assert apos[0] <= 32 * 1024
    GA = [MM[0], MM[1]]
    UB = [MM[2], S0]
    FB = [OB[0], OB[1]]
    GAn = ["MM0", "MM1"]
    UBn = ["MM2", "S0"]
    FBn = ["OB0", "OB1"]

    for g in range(NG):
        for b in range(4):
            i = 4 * g + b
            op("sp", lambda e, i=i, b=b: e.dma_start(out=x1g[:, b, :], in_=y_d[128 * i:128 * i + 128, :]),
               reads=["y:%d" % i], writes=["x1g%d" % b], dma="x1g%d" % b)
        for b in range(4):
            op("act", lambda e, b=b: e.activation(out=junk[:, 0:1024], in_=x1g[:, b, :], func=AF.Square,
                                                  accum_out=ss[:, 4 + b:5 + b]),
               reads=["x1g%d" % b], writes=["ss%d" % (4 + b), "junk"])
        for b in range(4):
            rms_rstd(None, 4 + b)
        for b in range(4):
            op("dve", lambda e, b=b: e.tensor_scalar(out=hb[:], in0=x1g[:, b, :], scalar1=rstd[:, 4 + b:5 + b],
                                                     scalar2=None, op0=ALU.mult),
               reads=["x1g%d" % b, "rstd%d" % (4 + b)], writes=["hb"])
            tb = b % 2
            for k in range(8):
                op("pe", lambda e, k=k, tb=tb: e.transpose(out=TB[tb][:, 128 * k:128 * k + 128],
                                                           in_=hb[:, 128 * k:128 * k + 128], identity=ident[:]),
                   reads=["hb", "ident"], writes=["T%d" % tb])
            for k in range(8):
                if True:
                    op("dve", lambda e, k=k, tb=tb, b=b: e.tensor_scalar(
                        out=h2T[:, k, 128 * b:128 * b + 128], in0=TB[tb][:, 128 * k:128 * k + 128],
                        scalar1=gcol[:, 8 + k:9 + k], scalar2=None, op0=ALU.mult),
                       reads=["T%d" % tb, "gcol"], writes=["h2T%d" % k])
                else:
                    op("act", lambda e, k=k, tb=tb, b=b: e.activation(
                        out=h2T[:, k, 128 * b:128 * b + 128], in_=TB[tb][:, 128 * k:128 * k + 128], func=AF.Identity,
                        scale=gcol[:, 8 + k:9 + k]),
                       reads=["T%d" % tb, "gcol"], writes=["h2T%d" % k])
        for c in range(NFF):
            j = c % 2
            for k in range(8):
                op("pe", lambda e, k=k, c=c, j=j: e.matmul(GA[j][:, :], lhsT=wg[:, k, 128 * c:128 * c + 128], rhs=h2T[:, k, :],
                                                           start=(k == 0), stop=(k == 7)),
                   reads=["wg_%d_%d" % (k, 0 if c < 11 else 1), "h2T%d" % k], writes=[GAn[j]])
            for k in range(8):
                op("pe", lambda e, k=k, c=c, j=j: e.matmul(UB[j][:, :], lhsT=wu[:, k, 128 * c:128 * c + 128], rhs=h2T[:, k, :],
                                                           start=(k == 0), stop=(k == 7)),
                   reads=["wu_%d_%d" % (k, 0 if c < 11 else 1), "h2T%d" % k], writes=[UBn[j]])
            op("act", lambda e, j=j: e.activation(out=sg[j][:], in_=GA[j][:, :], func=AF.Silu),
               reads=[GAn[j]], writes=["sg%d" % j])
            op("dve", lambda e, j=j, c=c: e.tensor_tensor(out=actT[:, c, :], in0=sg[j][:], in1=UB[j][:, :], op=ALU.mult),
               reads=["sg%d" % j, UBn[j]], writes=["actT%d" % c])
        for b in range(4):
            i = 4 * g + b
            for og in range(2):
                for c in range(NFF):
                    op("pe", lambda e, c=c, og=og, b=b: e.matmul(
                        FB[og][:, :], lhsT=actT[:, c, 128 * b:128 * b + 128], rhs=wd[:, c, 512 * og:512 * og + 512],
                        start=(c == 0), stop=(c == NFF - 1)),
                       reads=["actT%d" % c, "wd_%d" % c], writes=[FBn[og]])
            op("act", lambda e: e.activation(out=junk[:, 0:512], in_=FB[0][:, :], func=AF.Square, accum_out=ss[:, 2:3]),
               reads=["OB0"], writes=["ssa", "junk"])
            op("act", lambda e: e.activation(out=junk[:, 512:1024], in_=FB[1][:, :], func=AF.Square, accum_out=ss[:, 3:4]),
               reads=["OB1"], writes=["ssb", "junk"])
            op("dve", lambda e: e.tensor_tensor(out=ss[:, 1:2], in0=ss[:, 2:3], in1=ss[:, 3:4], op=ALU.add),
               reads=["ssa", "ssb"], writes=["ss1"])
            rms_rstd(None, 1)
            for og in range(2):
                op("dve", lambda e, og=og: e.scalar_tensor_tensor(
                    out=outx[:, 512 * og:512 * og + 512], in0=FB[og][:, :], scalar=rstd[:, 1:2],
                    in1=gpost[:, 512 * og:512 * og + 512], op0=ALU.mult, op1=ALU.mult),
                   reads=[FBn[og], "rstd1", "gpost"], writes=["outx%d" % og])
            op("dve", lambda e, b=b: e.tensor_tensor(out=outx[:], in0=outx[:], in1=x1g[:, b, :], op=ALU.add),
               reads=["outx0", "outx1", "x1g%d" % b], writes=["outx0", "outx1"])
            op("sp", lambda e, i=i: e.dma_start(out=y_d[128 * i:128 * i + 128, :], in_=outx[:]),
               reads=["outx0", "outx1"], writes=["y:%d" % i], dma="yst")
    return


def _t5_bucket(rel):
    rel = np.asarray(rel, np.int32)
    nb = 16
    ret = (rel > 0).astype(np.int32) * nb
    n = np.abs(rel)
    me = 8
    nf = np.maximum(n, 1).astype(np.float32)
    large = me + (np.log(nf / np.float32(me)) / np.float32(np.log(128 / me)) * np.float32(nb - me)).astype(np.int32)
    large = np.minimum(large, nb - 1)
    return ret + np.where(n < me, n, large)


def host_consts(inp):
    f32 = np.float32
    c = {}
    c["ident"] = np.eye(128, dtype=f32)
    pc = np.arange(128) // 64
    c["vis"] = np.where(pc[None, :] <= pc[:, None], 0.0, NEG).astype(f32)
    c["pw"] = np.broadcast_to((2.0 ** -np.arange(T_BIS + 1)).astype(f32), (128, T_BIS + 1)).copy()
    gcol = np.zeros((128, 16), f32)
    gcol[:, 0:8] = np.asarray(inp["g_pre_mix"], f32).reshape(8, 128).T
    gcol[:, 8:16] = np.asarray(inp["g_pre_ffn"], f32).reshape(8, 128).T
    c["gcol"] = gcol
    gpost = np.zeros((128, 2048), f32)
    gpost[:, 0:1024] = np.asarray(inp["g_post_mix"], f32).reshape(1, 1024)
    gpost[:, 1024:2048] = np.asarray(inp["g_post_ffn"], f32).reshape(1, 1024)
    c["gpost"] = gpost
    ln = np.zeros((128, 1024), f32)
    ln[:, 0:512] = np.asarray(inp["sgu_ln_g"], f32).reshape(1, 512)
    ln[:, 512:1024] = np.asarray(inp["sgu_ln_b"], f32).reshape(1, 512)
    c["lngb"] = ln
    rb = np.asarray(inp["rel_bias"], f32)
    p = np.arange(128)[:, None]
    j = np.arange(256)[None, :]
    bucket = _t5_bucket(j - 128 - p)
    bh = np.zeros((128, 4 * 256 + 4), f32)
    for h in range(4):
        bh[:, 256 * h:256 * h + 256] = rb[bucket, h]
        bh[:, 1024 + h] = rb[15, h]
    c["bh"] = bh
    sw = np.asarray(inp["sgu_w"], f32).reshape(4, 128, 128)
    c["wsT"] = np.ascontiguousarray(sw.transpose(2, 0, 1).reshape(128, 512))
    c["bsT"] = np.ascontiguousarray(np.asarray(inp["sgu_b"], f32).reshape(4, 128).T)
    return c


def make_in_maps(inputs, n_cores, S):
    c = host_consts(inputs)
    shared = dict(c)
    shared["w_in"] = np.ascontiguousarray(np.asarray(inputs["w_in"], np.float32).reshape(D_MODEL, 3144))
    shared["w_o"] = np.ascontiguousarray(np.asarray(inputs["w_o"], np.float32).reshape(D_MODEL, D_MODEL))
    shared["w_gate"] = np.ascontiguousarray(np.asarray(inputs["w_gate"], np.float32).reshape(D_MODEL, D_FF))
    shared["w_up"] = np.ascontiguousarray(np.asarray(inputs["w_up"], np.float32).reshape(D_MODEL, D_FF))
    shared["w_down"] = np.ascontiguousarray(np.asarray(inputs["w_down"], np.float32).reshape(D_FF, D_MODEL))
    x = np.asarray(inputs["x"], np.float32)
    maps = []
    for b in range(n_cores):
        m = dict(shared)
        m["x"] = np.ascontiguousarray(x[b, :S])
        maps.append(m)
    return maps


def kernel(**inputs):
    n = 8
    nc = build(NB=32, debug=False)
    in_maps = make_in_maps(inputs, n, SEQ)
    res = run_bass_kernel_spmd(nc, in_maps, core_ids=list(range(n)))
    out = np.stack([np.asarray(r["y"], np.float32) for r in res.results], axis=0)
    return out
```

```python
import numpy as np
from contextlib import ExitStack
import concourse.bass as bass
import concourse.mybir as mybir
from concourse.bass_utils import run_bass_kernel_spmd
from concourse.alu_op_type import AluOpType as ALU

F32 = mybir.dt.float32
BF16 = mybir.dt.bfloat16
AF = mybir.ActivationFunctionType
AX = mybir.AxisListType

D_MODEL = 1024
SEQ = 4096
D_FF = 2816
NFF = D_FF // 128
TOPK = 256
T_BIS = 14
EPS = 1e-6
NEG = -1.0e30
QSCALE = 128 ** -0.5
ISCALE = (64 ** -0.5) * (8 ** -0.5)


class _Stop(Exception):
    pass


class Sched:
    def __init__(self, nc, es):
        self.nc = nc
        self.es = es
        self.eng = {"pe": nc.tensor, "act": nc.scalar, "dve": nc.vector, "pool": nc.gpsimd, "sp": nc.sync}
        self.semh = {}
        self.cnt = {}
        for e in self.eng:
            self.semh[e] = es.enter_context(nc.semaphore("sem_" + e))
            self.cnt[e] = 0
        self.seen = {e: {} for e in self.eng}
        self.lastw = {}
        self.readers = {}
        self.nwait = 0
        self.nops = 0
        self.stop_ops = None
        self.log = None
        self.rec = None

    def _dma_key(self, slot):
        key = "dma:" + slot
        if key not in self.semh:
            self.semh[key] = self.es.enter_context(self.nc.semaphore("sd_" + slot))
            self.cnt[key] = 0
        return key

    PSUM_RES = frozenset(["MM0", "MM1", "MM2", "S0", "OB0", "OB1", "T0", "T1"])

    def record(self, f):
        saved = self.rec
        self.rec = []
        f()
        out = self.rec
        self.rec = saved
        return out

    def interleave(self, la, lb):
        na, nb = len(la), len(lb)
        ia = ib = 0
        import os
        if os.environ.get("NOINTER"):
            for a in la: self.op(*a)
            for b in lb: self.op(*b)
            return
        while ia < na or ib < nb:
            if ib >= nb or (ia < na and ia * nb <= ib * na):
                self.op(*la[ia])
                ia += 1
            else:
                self.op(*lb[ib])
                ib += 1

    def op(self, eng, fn, reads=(), writes=(), dma=None):
        if self.rec is not None:
            self.rec.append((eng, fn, tuple(reads), tuple(writes), dma))
            return None
        pr = [r for r in reads if r in self.PSUM_RES]
        if pr:
            writes = list(writes) + pr
        deps = {}

        def add(d, raw):
            if d is None:
                return
            key, val, deng = d
            if deng == eng and not key.startswith("dma:"):
                if eng in ("pe", "sp"):
                    return
            if deps.get(key, 0) < val:
                deps[key] = val

        for r in reads:
            add(self.lastw.get(r), True)
        for w in writes:
            add(self.lastw.get(w), False)
            for key, (val, deng) in self.readers.get(w, {}).items():
                add((key, val, deng), False)
        E = self.eng[eng]
        for key, val in deps.items():
            if self.seen[eng].get(key, 0) < val:
                E.wait_ge(self.semh[key], val)
                self.seen[eng][key] = val
                self.nwait += 1
        if self.stop_ops is not None and self.nops >= self.stop_ops:
            raise _Stop()
        inst = fn(E)
        if self.log is not None:
            import sys as _s
            self.log.append((self.nops, eng, _s._getframe(1).f_lineno, tuple(reads), tuple(writes)))
        self.nops += 1
        if dma is not None:
            key = self._dma_key(dma)
            self.cnt[key] += 16
            inst.then_inc(self.semh[key], 16)
        else:
            key = eng
            self.cnt[key] += 1
            inst.then_inc(self.semh[key], 1)
        val = self.cnt[key]
        for r in reads:
            d = self.readers.setdefault(r, {})
            if d.get(key, (0, None))[0] < val:
                d[key] = (val, eng)
        for w in writes:
            self.lastw[w] = (key, val, eng)
            self.readers[w] = {}
        return inst

    def regroup(self, slot, names):
        key = "dma:" + slot
        for n in names:
            k0, v0, e0 = self.lastw[n]
            self.lastw[n] = (key, self.cnt[key], e0)

    def barrier(self):
        for e in self.eng:
            E = self.eng[e]
            for key, val in self.cnt.items():
                if val > 0 and key != e and self.seen[e].get(key, 0) < val:
                    E.wait_ge(self.semh[key], val)
                    self.seen[e][key] = val

    def final_wait(self, eng="sp"):
        E = self.eng[eng]
        for key, val in self.cnt.items():
            if val > 0 and key != eng and self.seen[eng].get(key, 0) < val:
                E.wait_ge(self.semh[key], val)
                self.seen[eng][key] = val


def skew_emit(items, nstage):
    n = len(items)
    for step in range(n + nstage - 1):
        for s in range(nstage):
            j = step - s
            if 0 <= j < n:
                items[j][s]()


def build(NB=32, debug=False, stop=None, stop_ops=None):
    nc = bass.Bass("TRN2", target_bir_lowering=False)
    es = ExitStack()
    sc = Sched(nc, es)
    sc.stop_ops = stop_ops
    try:
        _build_body(nc, es, sc, NB, debug, stop)
    except _Stop:
        pass
    sc.final_wait("sp")
    es.close()
    nc._sched_stats = (sc.nops, sc.nwait)
    return nc


def _build_body(nc, es, sc, NB, debug, stop):
    def check(tag):
        if stop == tag:
            raise _Stop()

    S = NB * 128
    NG = NB // 4
    dt = nc.dram_tensor
    x_d = dt("x", [S, D_MODEL], F32, kind="ExternalInput").ap()
    w_in_d = dt("w_in", [D_MODEL, 3144], F32, kind="ExternalInput").ap()
    w_o_d = dt("w_o", [D_MODEL, D_MODEL], F32, kind="ExternalInput").ap()
    w_g_d = dt("w_gate", [D_MODEL, D_FF], F32, kind="ExternalInput").ap()
    w_u_d = dt("w_up", [D_MODEL, D_FF], F32, kind="ExternalInput").ap()
    w_d_d = dt("w_down", [D_FF, D_MODEL], F32, kind="ExternalInput").ap()
    ident_d = dt("ident", [128, 128], F32, kind="ExternalInput").ap()
    vis_d = dt("vis", [128, 128], F32, kind="ExternalInput").ap()
    pw_d = dt("pw", [128, T_BIS + 1], F32, kind="ExternalInput").ap()
    gcol_d = dt("gcol", [128, 16], F32, kind="ExternalInput").ap()
    gpost_d = dt("gpost", [128, 2048], F32, kind="ExternalInput").ap()
    ln_d = dt("lngb", [128, 1024], F32, kind="ExternalInput").ap()
    bh_d = dt("bh", [128, 4 * 256 + 4], F32, kind="ExternalInput").ap()
    wsT_d = dt("wsT", [128, 512], F32, kind="ExternalInput").ap()
    bs_d = dt("bsT", [128, 4], F32, kind="ExternalInput").ap()
    y_d = dt("y", [S, D_MODEL], F32, kind="ExternalOutput").ap()
    if debug:
        dbg_ob = dt("dbg_ob", [S, 512], BF16, kind="ExternalOutput").ap()
        dbg_sc = dt("dbg_sc", [128, 4096], F32, kind="ExternalOutput").ap()
        dbg_thr = dt("dbg_thr", [128, NB], F32, kind="ExternalOutput").ap()
        dbg_x1 = dt("dbg_x1", [S, D_MODEL], F32, kind="ExternalOutput").ap()

    op = sc.op

    def sb(name, shape, dtype, stack=es):
        return stack.enter_context(nc.sbuf_tensor("s_" + name, shape, dtype))

    def ps(name, shape, dtype, stack=es):
        return stack.enter_context(nc.psum_tensor("p_" + name, shape, dtype))

    ARENA_BYTES = 198 * 1024
    arena = es.enter_context(nc.sbuf_tensor("s_arena", [128, ARENA_BYTES // 2], BF16))
    apos = [0]

    def aset(off_bytes):
        apos[0] = off_bytes

    def av(name, shape, dtype, stack=None):
        esz = 4 if dtype == F32 else 2
        nel = 1
        for d in shape[1:]:
            nel *= d
        nbytes = (nel * esz + 63) // 64 * 64
        off = apos[0]
        assert off % 64 == 0 and off + nbytes <= ARENA_BYTES, (name, off, nbytes)
        apos[0] = off + nbytes
        v = arena[:, off // 2:off // 2 + nel * esz // 2]
        if dtype == F32:
            v = v.bitcast(F32)
        if len(shape) == 3:
            v = v.rearrange("p (a b) -> p a b", a=shape[1])
        elif len(shape) == 4:
            v = v.rearrange("p (a b c) -> p a b c", a=shape[1], b=shape[2])
        return v

    T0 = ps("T0", [128, 1024], BF16)
    T1 = ps("T1", [128, 1024], BF16)
    MM = [ps("MM%d" % i, [128, 512], F32) for i in range(3)]
    S0 = ps("S0", [128, 512], F32)
    OB = [ps("OB%d" % i, [128, 512], F32) for i in range(2)]
    TB = [T0, T1]
    LGB = [(MM[2][:, :], "MM2"), (T1[:, :].bitcast(F32), "T1")]

    ident = sb("ident", [128, 128], BF16)
    gcol = sb("gcol", [128, 16], F32)
    junk = sb("junk", [128, 1024], BF16)
    ss = sb("ss", [128, 8], F32)
    rstd = sb("rstd", [128, 8], F32)
    hb = sb("hb", [128, 1024], BF16)
    epsb = sb("epsb", [128, 2], F32)
    aset(0)
    outb = av("outb", [128, 32, 512], BF16)
    xs_main = av("xs", [128, 1024], F32)
    hT = av("hT", [128, 1024], BF16)
    P1A_BASE = 38 * 1024
    aset(P1A_BASE)
    sb = av
    p1a = None
    vis = sb("vis", [128, 128], F32, p1a)
    pw = sb("pw", [128, T_BIS + 1], F32, p1a)
    bh = sb("bh", [128, 4 * 256 + 4], F32, p1a)

    op("pool", lambda e: e.dma_start(out=ident[:], in_=ident_d[:, :]), writes=["ident"], dma="c_ident")
    op("sp", lambda e: e.dma_start(out=vis[:], in_=vis_d[:, :]), writes=["vis"], dma="c0")
    op("sp", lambda e: e.dma_start(out=pw[:], in_=pw_d[:, :]), writes=["pw"], dma="c1")
    op("sp", lambda e: e.dma_start(out=gcol[:], in_=gcol_d[:, :]), writes=["gcol"], dma="c2")
    op("sp", lambda e: e.dma_start(out=bh[:], in_=bh_d[:, :]), writes=["bh"], dma="c4")

    def rms_rstd(src_ops, col):
        op("act", lambda e: e.activation(out=rstd[:, col:col + 1], in_=ss[:, col:col + 1], func=AF.Ln,
                                         bias=epsb[:, 0:1], scale=1.0 / 1024),
           reads=["ss%d" % col, "epsb"], writes=["rstd%d" % col])
        op("act", lambda e: e.activation(out=rstd[:, col:col + 1], in_=rstd[:, col:col + 1], func=AF.Exp,
                                         scale=-0.5),
           reads=["rstd%d" % col], writes=["rstd%d" % col])

    op("dve", lambda e: e.memset(epsb[:, 0:1], EPS), writes=["epsb"])
    check("consts")

    def load_norm_transpose(i, gc0, xs=None, xn="xs"):
        if xs is None:
            xs = xs_main
        t0 = i * 128
        op("sp", lambda e: e.dma_start(out=xs[:], in_=x_d[t0:t0 + 128, :]), writes=[xn], dma=xn)
        op("act", lambda e: e.activation(out=junk[:, 0:1024], in_=xs[:], func=AF.Square, accum_out=ss[:, 0:1]),
           reads=[xn], writes=["ss0", "junk"])
        rms_rstd(None, 0)
        op("dve", lambda e: e.tensor_scalar(out=hb[:], in0=xs[:], scalar1=rstd[:, 0:1], scalar2=None, op0=ALU.mult),
           reads=[xn, "rstd0"], writes=["hb"])
        for k in range(8):
            op("pe", lambda e, k=k: e.transpose(out=T0[:, 128 * k:128 * k + 128], in_=hb[:, 128 * k:128 * k + 128],
                                                identity=ident[:]),
               reads=["hb", "ident"], writes=["T0"])
        for k in range(8):
            if True:
                op("dve", lambda e, k=k: e.tensor_scalar(out=hT[:, 128 * k:128 * k + 128], in0=T0[:, 128 * k:128 * k + 128],
                                                         scalar1=gcol[:, gc0 + k:gc0 + k + 1], scalar2=None, op0=ALU.mult),
                   reads=["T0", "gcol"], writes=["hT%d" % k])
            else:
                op("act", lambda e, k=k: e.activation(out=hT[:, 128 * k:128 * k + 128], in_=T0[:, 128 * k:128 * k + 128],
                                                      func=AF.Identity, scale=gcol[:, gc0 + k:gc0 + k + 1]),
                   reads=["T0", "gcol"], writes=["hT%d" % k])

    w1 = sb("w1", [128, 8, 2120], BF16, p1a)
    kT = sb("kT", [128, 4, S], BF16, p1a)
    vc = sb("vc", [128, NB, 4, 130], BF16, p1a)
    ikT = sb("ikT", [128, S], BF16, p1a)
    score = sb("score", [128, S], F32, p1a)
    sel = sb("sel", [128, S], BF16, p1a)
    q_sb = sb("q_sb", [128, 512], BF16, p1a)
    k_sb = sb("k_sb", [128, 512], BF16, p1a)
    iq_sb = sb("iq_sb", [128, 512], BF16, p1a)
    ik2 = sb("ik2", [128, 128], BF16, p1a)
    qT2 = [sb("qT%d" % j, [128, 512], BF16, p1a) for j in range(2)]
    iqT2 = [sb("iqT%d" % j, [128, 512], BF16, p1a) for j in range(2)]
    Dg2 = [sb("Dg%d" % j, [128, 8, 128], BF16, p1a) for j in range(2)]
    iwc2 = [sb("iwc%d" % j, [128, 8], F32, p1a) for j in range(2)]
    junkd = sb("junkd", [128, 512], BF16, p1a)
    rbuf = [sb("r%d" % j, [128, 512], BF16, p1a) for j in range(3)]
    Eb = [sb("E%d" % j, [128, 512], BF16, p1a) for j in range(2)]
    Pb = [sb("P%d" % j, [128, 512], BF16, p1a) for j in range(2)]
    PTb = [sb("PT%d" % j, [128, 512], BF16, p1a) for j in range(2)]
    tmpb = sb("tmpb", [128, 256], F32, p1a)
    amaxp = sb("amaxp", [128, 8], F32, p1a)
    aminp = sb("aminp", [128, 8], F32, p1a)
    bis = sb("bis", [128, 8], F32, p1a)
    D2 = sb("D2", [128, T_BIS + 1], F32, p1a)
    thr_all = sb("thr_all", [128, NB], F32, p1a)

    for k in range(8):
        op("pool", lambda e, k=k: e.dma_start(out=w1[:, k, :], in_=w_in_d[128 * k:128 * k + 128, 1024:3144],
                                              max_dma_last_dim=4096),
           writes=["w1_%d" % k], dma="w1")
    sc.regroup("w1", ["w1_%d" % k for k in range(8)])
    op("dve", lambda e: e.memset(vc[:, :, :, 128:130], 1.0), writes=["vc_ones"])

    dots_rot = [0]
    lg_rot = [0]
    e_rot = [0]
    tb_rot = [0]

    def p1a_block(i, st):
        t0 = i * 128
        L = t0 + 128
        par = i % 2
        nkt = (L + 511) // 512
        qT, iqT, Dg, iwc = qT2[par], iqT2[par], Dg2[par], iwc2[par]
        nqT, niqT, niwc = "qT_%d" % par, "iqT_%d" % par, "iwc_%d" % par
        if st == "A":
            load_norm_transpose(i, 0)
            groups = [("q", 0, 512), ("k", 512, 512), ("v", 1024, 512), ("iq", 1536, 512), ("ik", 2048, 72)]
            for gi, (nm, c0, w) in enumerate(groups):
                bank = MM[gi % 3]
                bname = "MM%d" % (gi % 3)
                for k in range(8):
                    op("pe", lambda e, k=k, bank=bank, c0=c0, w=w: e.matmul(
                        bank[:, 0:w], lhsT=hT[:, 128 * k:128 * k + 128], rhs=w1[:, k, c0:c0 + w],
                        start=(k == 0), stop=(k == 7)),
                       reads=["hT%d" % k, "w1_%d" % k], writes=[bname])
                if nm == "q":
                    op("act", lambda e, bank=bank: e.activation(out=q_sb[:], in_=bank[:, :], func=AF.Copy),
                       reads=[bname], writes=["q_sb"])
                elif nm == "k":
                    op("dve", lambda e, bank=bank: e.tensor_copy(out=k_sb[:], in_=bank[:, :]),
                       reads=[bname], writes=["k_sb"])
                elif nm == "v":
                    op("act", lambda e, bank=bank, i=i: e.activation(
                        out=vc[:, i, :, 0:128], in_=bank[:, :].rearrange("p (h d) -> p h d", h=4), func=AF.Copy),
                       reads=[bname], writes=["vc:%d" % i])
                elif nm == "iq":
                    op("dve", lambda e, bank=bank: e.tensor_copy(out=iq_sb[:], in_=bank[:, :]),
                       reads=[bname], writes=["iq_sb"])
                else:
                    op("act", lambda e, bank=bank: e.activation(out=ik2[:, 0:64], in_=bank[:, 0:64], func=AF.Copy),
                       reads=[bname], writes=["ik2a"])
                    op("dve", lambda e, bank=bank: e.tensor_copy(out=ik2[:, 64:128], in_=bank[:, 0:64]),
                       reads=[bname], writes=["ik2b"])
                    op("dve", lambda e, bank=bank: e.tensor_scalar(out=iwc[:], in0=bank[:, 64:72], scalar1=ISCALE,
                                                                   scalar2=None, op0=ALU.mult),
                       reads=[bname], writes=[niwc])
            for h in range(4):
                op("pe", lambda e, h=h: e.transpose(out=T1[:, 128 * h:128 * h + 128], in_=q_sb[:, 128 * h:128 * h + 128],
                                                    identity=ident[:]),
                   reads=["q_sb", "ident"], writes=["T1"])
            for h in range(4):
                op("pe", lambda e, h=h: e.transpose(out=T1[:, 512 + 128 * h:512 + 128 * h + 128],
                                                    in_=k_sb[:, 128 * h:128 * h + 128], identity=ident[:]),
                   reads=["k_sb", "ident"], writes=["T1"])
            op("dve", lambda e: e.tensor_copy(out=qT[:], in_=T1[:, 0:512]), reads=["T1"], writes=[nqT])
            op("dve", lambda e, t0=t0: e.tensor_copy(out=kT[:, :, t0:t0 + 128],
                                                     in_=T1[:, 512:1024].rearrange("p (h t) -> p h t", h=4)),
               reads=["T1"], writes=["kT:%d" % i])
            for h in range(4):
                op("pe", lambda e, h=h: e.transpose(out=T0[:, 128 * h:128 * h + 128], in_=iq_sb[:, 128 * h:128 * h + 128],
                                                    identity=ident[:]),
                   reads=["iq_sb", "ident"], writes=["T0"])
            op("pe", lambda e: e.transpose(out=T0[:, 512:640], in_=ik2[:, :], identity=ident[:]),
               reads=["ik2a", "ik2b", "ident"], writes=["T0"])
            op("dve", lambda e: e.tensor_copy(out=iqT[:], in_=T0[:, 0:512]), reads=["T0"], writes=[niqT])
            op("dve", lambda e, t0=t0: e.tensor_copy(out=ikT[:, t0:t0 + 128], in_=T0[:, 512:640]),
               reads=["T0"], writes=["ikT:%d" % i])
            for h in range(8):
                op("dve", lambda e, h=h: e.tensor_scalar(out=Dg[:, h, :], in0=ident[:], scalar1=iwc[:, h:h + 1],
                                                          scalar2=1.0, op0=ALU.mult, op1=ALU.mult),
                   reads=["ident", niwc], writes=["Dg%d_%d" % (par, h)])

        if st == "I":
            pairs = [(kt, h) for kt in range(nkt) for h in range(8)]

            def emit_dots(n):
                kt, h = pairs[n]
                N = min(512, L - 512 * kt)
                b = dots_rot[0] % 2
                dots_rot[0] += 1
                pb = 64 * (h % 2)
                cb = 128 * (h // 2)
                blks = ["ikT:%d" % bb for bb in range(4 * kt, min(4 * kt + 4, i + 1))]
                op("pe", lambda e: e.matmul(MM[b][:, 0:N], lhsT=iqT[pb:pb + 64, cb:cb + 128],
                                            rhs=ikT[pb:pb + 64, 512 * kt:512 * kt + N], start=True, stop=True),
                   reads=[niqT] + blks, writes=["MM%d" % b])
                return b

            dbank = {}
            for n in range(min(2, len(pairs))):
                dbank[n] = emit_dots(n)
            for n, (kt, h) in enumerate(pairs):
                N = min(512, L - 512 * kt)
                b = dbank[n]
                rj = n % 3
                if h % 2 == 0:
                    op("act", lambda e, b=b, rj=rj, N=N: e.activation(out=rbuf[rj][:, 0:N], in_=MM[b][:, 0:N], func=AF.Relu),
                       reads=["MM%d" % b], writes=["r%d" % rj])
                else:
                    op("dve", lambda e, b=b, rj=rj, N=N: e.tensor_scalar(out=rbuf[rj][:, 0:N], in0=MM[b][:, 0:N], scalar1=0.0,
                                                                         scalar2=None, op0=ALU.max),
                       reads=["MM%d" % b], writes=["r%d" % rj])
                op("pe", lambda e, rj=rj, N=N, h=h: e.matmul(S0[:, 0:N], lhsT=Dg[:, h, :], rhs=rbuf[rj][:, 0:N],
                                                             start=(h == 0), stop=(h == 7)),
                   reads=["r%d" % rj, "Dg%d_%d" % (par, h)], writes=["S0"])
                if n + 2 < len(pairs):
                    dbank[n + 2] = emit_dots(n + 2)
                if h == 7:
                    op("act", lambda e, kt=kt, N=N: e.activation(out=score[:, 512 * kt:512 * kt + N], in_=S0[:, 0:N],
                                                                 func=AF.Copy),
                       reads=["S0"], writes=["score"])
                    op("dve", lambda e, kt=kt, N=N: e.tensor_scalar(
                        out=junkd[:, 0:N], in0=score[:, 512 * kt:512 * kt + N], scalar1=-3.0e38, scalar2=None,
                        op0=ALU.max, op1=ALU.max, accum_out=amaxp[:, kt:kt + 1]),
                       reads=["score"], writes=["amaxp", "junkd"])
                    op("dve", lambda e, kt=kt, N=N: e.tensor_scalar(
                        out=junkd[:, 0:N], in0=score[:, 512 * kt:512 * kt + N], scalar1=3.0e38, scalar2=None,
                        op0=ALU.min, op1=ALU.min, accum_out=aminp[:, kt:kt + 1]),
                       reads=["score"], writes=["aminp", "junkd"])
            op("dve", lambda e, L=L: e.tensor_tensor(out=score[:, L - 128:L], in0=score[:, L - 128:L], in1=vis[:],
                                                      op=ALU.add),
               reads=["score", "vis"], writes=["score"])

        if st == "B":
            A_, MID, CNT, PM, THR = (bis[:, j:j + 1] for j in range(5))
            if L > TOPK:
                op("dve", lambda e: e.tensor_scalar(out=amaxp[:, 0:nkt], in0=amaxp[:, 0:nkt], scalar1=-3.0e38, scalar2=None,
                                                    op0=ALU.max, op1=ALU.max, accum_out=bis[:, 6:7]),
                   reads=["amaxp"], writes=["amaxp", "bis6"])
                op("dve", lambda e: e.tensor_scalar(out=aminp[:, 0:nkt], in0=aminp[:, 0:nkt], scalar1=3.0e38, scalar2=None,
                                                    op0=ALU.min, op1=ALU.min, accum_out=bis[:, 7:8]),
                   reads=["aminp"], writes=["aminp", "bis7"])
                op("dve", lambda e: e.scalar_tensor_tensor(out=A_, in0=bis[:, 7:8], scalar=-1.0, in1=bis[:, 6:7],
                                                           op0=ALU.mult, op1=ALU.max),
                   reads=["bis6", "bis7"], writes=["bisA"])
                op("dve", lambda e: e.tensor_scalar(out=D2[:], in0=pw[:], scalar1=A_, scalar2=None, op0=ALU.mult),
                   reads=["bisA", "pw"], writes=["D2"])
                op("dve", lambda e: e.memset(MID, 0.0), writes=["mid"])
                Ld = L if L < 1024 else ((L * 9 // 20 + 127) // 128) * 128
                nA = L - Ld
                for k in range(T_BIS):
                    op("dve", lambda e: e.tensor_scalar(out=sel[:, 0:Ld], in0=score[:, 0:Ld], scalar1=MID, scalar2=None,
                                                        op0=ALU.is_gt, op1=ALU.add, accum_out=CNT),
                       reads=["score", "mid"], writes=["cnt", "sel"])
                    if nA > 0:
                        op("act", lambda e: e.activation(out=sel[:, Ld:L], in_=score[:, Ld:L], func=AF.Sign,
                                                         bias=MID, scale=-1.0, accum_out=bis[:, 6:7]),
                           reads=["score", "mid"], writes=["bis6", "selB"])
                        op("dve", lambda e: e.scalar_tensor_tensor(out=CNT, in0=bis[:, 6:7], scalar=-0.5, in1=CNT,
                                                                   op0=ALU.mult, op1=ALU.add),
                           reads=["bis6", "cnt"], writes=["cnt"])
                    op("dve", lambda e: e.tensor_scalar(out=PM, in0=CNT, scalar1=TOPK - 0.5 - nA / 2.0, scalar2=0.5,
                                                        op0=ALU.is_gt, op1=ALU.subtract),
                       reads=["cnt"], writes=["pm"])
                    op("dve", lambda e, k=k: e.scalar_tensor_tensor(out=MID, in0=PM, scalar=D2[:, k:k + 1], in1=MID,
                                                                    op0=ALU.mult, op1=ALU.add),
                       reads=["pm", "D2", "mid"], writes=["mid"])
                op("dve", lambda e: e.tensor_tensor(out=THR, in0=MID, in1=D2[:, T_BIS:T_BIS + 1], op=ALU.subtract),
                   reads=["mid", "D2"], writes=["thr"])
            else:
                op("dve", lambda e: e.memset(THR, -1.0e29), writes=["thr"])
            if debug:
                op("dve", lambda e, i=i: e.tensor_copy(out=thr_all[:, i:i + 1], in_=THR), reads=["thr"], writes=["thr_all"])
            op("dve", lambda e, L=L: e.tensor_scalar(out=sel[:, 0:L], in0=score[:, 0:L], scalar1=THR, scalar2=None,
                                                     op0=ALU.is_gt),
               reads=["score", "thr"], writes=["sel", "selB"])

        if st == "C":
            ntile = (L + 511) // 512
            bw = min(256, L)
            items = []
            for h in range(4):
                for j in range(ntile - 1, -1, -1):
                    c1 = L - 512 * j
                    c0 = max(0, c1 - 512)
                    N = c1 - c0
                    first = (j == ntile - 1)
                    last = (j == 0)
                    st = {}

                    def s1(h=h, c0=c0, c1=c1, N=N, j=j, st=st):
                        LG, lgn = LGB[lg_rot[0] % 2]
                        lg_rot[0] += 1
                        ej = e_rot[0] % 2
                        e_rot[0] += 1
                        st["ej"] = ej
                        blks = ["kT:%d" % bb for bb in range(c0 // 128, c1 // 128)]
                        op("pe", lambda e: e.matmul(LG[:, 0:N], lhsT=qT[:, 128 * h:128 * h + 128], rhs=kT[:, h, c0:c1],
                                                    start=True, stop=True),
                           reads=[nqT] + blks, writes=[lgn])
                        nb_ = bw if j == 0 else 0
                        nf = N - nb_
                        if nf > 0:
                            op("act", lambda e: e.activation(out=Eb[ej][:, 0:nf], in_=LG[:, 0:nf], func=AF.Exp,
                                                             bias=bh[:, 1024 + h:1025 + h], scale=QSCALE),
                               reads=[lgn, "bh"], writes=["E%d" % ej])
                        if nb_ > 0:
                            op("dve", lambda e: e.scalar_tensor_tensor(
                                out=tmpb[:, 0:nb_], in0=LG[:, nf:N], scalar=QSCALE,
                                in1=bh[:, 256 * h + 256 - nb_:256 * h + 256], op0=ALU.mult, op1=ALU.add),
                               reads=[lgn, "bh"], writes=["tmpb"])
                            op("act", lambda e: e.activation(out=Eb[ej][:, nf:N], in_=tmpb[:, 0:nb_], func=AF.Exp),
                               reads=["tmpb"], writes=["E%d" % ej])
                        op("dve", lambda e: e.tensor_tensor(out=Pb[ej][:, 0:N], in0=Eb[ej][:, 0:N], in1=sel[:, c0:c1],
                                                            op=ALU.mult),
                           reads=["E%d" % ej, "sel", "selB"], writes=["P%d" % ej])

                    def s2(N=N, st=st):
                        ej = st["ej"]
                        tb = 0
                        pj = tb_rot[0] % 2
                        tb_rot[0] += 1
                        st["pj"] = pj
                        st["tb"] = tb
                        for m in range(N // 128):
                            op("pe", lambda e, m=m: e.transpose(out=TB[tb][:, 128 * m:128 * m + 128],
                                                                in_=Pb[ej][:, 128 * m:128 * m + 128], identity=ident[:]),
                               reads=["P%d" % ej, "ident"], writes=["T%d" % tb])
                        eng = "dve"
                        if eng == "act":
                            op("act", lambda e: e.activation(out=PTb[pj][:, 0:N], in_=TB[tb][:, 0:N], func=AF.Copy),
                               reads=["T%d" % tb], writes=["PT%d" % pj])
                        else:
                            op("dve", lambda e: e.tensor_copy(out=PTb[pj][:, 0:N], in_=TB[tb][:, 0:N]),
                               reads=["T%d" % tb], writes=["PT%d" % pj])

                    def s3(h=h, c0=c0, N=N, first=first, last=last, st=st, i=i):
                        pj = st["pj"]
                        ob = OB[h // 2]
                        off = 130 * (h % 2)
                        nm = N // 128
                        for m in range(nm):
                            blk = c0 // 128 + m
                            op("pe", lambda e, m=m, blk=blk: e.matmul(
                                ob[:, off:off + 130], lhsT=PTb[pj][:, 128 * m:128 * m + 128], rhs=vc[:, blk, h, :],
                                start=(first and m == 0), stop=(last and m == nm - 1)),
                               reads=["PT%d" % pj, "vc:%d" % blk, "vc_ones"], writes=["OB%d" % (h // 2)])
                        if last:
                            RS = bis[:, 5:6]
                            op("dve", lambda e: e.reciprocal(out=RS, in_=ob[:, off + 128:off + 129]),
                               reads=["OB%d" % (h // 2)], writes=["rs"])
                            op("dve", lambda e: e.tensor_scalar(out=outb[:, i, 128 * h:128 * h + 128], in0=ob[:, off:off + 128],
                                                                scalar1=RS, scalar2=None, op0=ALU.mult),
                               reads=["OB%d" % (h // 2), "rs"], writes=["outb:%d" % i])

                    items.append([s1, s2, s3])
            skew_emit(items, 3)

    rec = sc.record
    p1a_block(0, 'A')
    p1a_block(0, 'I')
    for i in range(NB):
        la = rec(lambda: p1a_block(i, 'B'))
        lb = rec(lambda: p1a_block(i + 1, 'A')) if i + 1 < NB else []
        sc.interleave(la, lb)
        la = rec(lambda: p1a_block(i, 'C'))
        lb = rec(lambda: p1a_block(i + 1, 'I')) if i + 1 < NB else []
        sc.interleave(la, lb)
        check("C%d" % i)

    if debug:
        op("sp", lambda e: e.dma_start(out=dbg_sc[:, 0:S], in_=score[:, 0:S]), reads=["score"], dma="dbg0")
        op("sp", lambda e: e.dma_start(out=dbg_thr[:, :], in_=thr_all[:]), reads=["thr_all"], dma="dbg1")
        for i in range(NB):
            op("sp", lambda e, i=i: e.dma_start(out=dbg_ob[128 * i:128 * i + 128, :], in_=outb[:, i, :]),
               reads=["outb:%d" % i], dma="dbg2")
    sc.barrier()

    check("P1a")
    aset(P1A_BASE)
    wg = sb("wg", [128, 8, D_FF], BF16)
    wu = sb("wu", [128, 8, D_FF], BF16)
    P1B_BASE = apos[0]
    p1b = None
    w1b = sb("w1b", [128, 8, 1024], BF16, p1b)
    wo = sb("wo", [128, 8, 1024], BF16, p1b)
    lngb = sb("lngb", [128, 1024], F32, p1b)
    wsT = sb("wsT", [128, 512], BF16, p1b)
    bsT = sb("bsT", [128, 4], F32, p1b)
    u_sb = sb("u_sb", [128, 512], F32, p1b)
    avb = sb("av", [128, 512], F32, p1b)
    vn = sb("vn", [128, 512], BF16, p1b)
    cata = sb("cata", [128, 512], BF16, p1b)
    catT = sb("catT", [128, 1024], BF16, p1b)
    tmpx = sb("tmpx", [128, 1024], F32, p1b)
    bst = sb("bst", [128, 8], F32, p1b)
    mv = sb("mv", [128, 8], F32, p1b)
    gpost = sb("gpost1", [128, 1024], F32, p1b)
    op("sp", lambda e: e.dma_start(out=gpost[:], in_=gpost_d[:, 0:1024]), writes=["gpost"], dma="c3")

    for k in range(8):
        op("pool", lambda e, k=k: e.dma_start(out=w1b[:, k, :], in_=w_in_d[128 * k:128 * k + 128, 0:1024]),
           writes=["w1b_%d" % k], dma="wp1b")
    for k in range(8):
        op("pool", lambda e, k=k: e.dma_start(out=wo[:, k, :], in_=w_o_d[128 * k:128 * k + 128, :]),
           writes=["wo_%d" % k], dma="wp1b")
    sc.regroup("wp1b", ["w1b_%d" % k for k in range(8)] + ["wo_%d" % k for k in range(8)])
    op("sp", lambda e: e.dma_start(out=lngb[:], in_=ln_d[:, :]), writes=["lngb"], dma="c0")
    op("pool", lambda e: e.dma_start(out=wsT[:], in_=wsT_d[:, :]), writes=["wsT"], dma="c_ident")
    op("sp", lambda e: e.dma_start(out=bsT[:], in_=bs_d[:, :]), writes=["bsT"], dma="c1")
    for g in range(4):
        op("dve", lambda e, g=g: e.memset(wsT[64:128, 128 * g:128 * g + 64], 0.0), reads=["wsT"], writes=["wsT"])

    ffn_loads = []
    for k in range(8):
        for half in range(2):
            c0 = half * 1408
            ffn_loads.append(lambda k=k, c0=c0, half=half: op(
                "pool", lambda e: e.dma_start(out=wg[:, k, c0:c0 + 1408], in_=w_g_d[128 * k:128 * k + 128, c0:c0 + 1408]),
                writes=["wg_%d_%d" % (k, half)], dma="wgu"))
            ffn_loads.append(lambda k=k, c0=c0, half=half: op(
                "pool", lambda e: e.dma_start(out=wu[:, k, c0:c0 + 1408], in_=w_u_d[128 * k:128 * k + 128, c0:c0 + 1408]),
                writes=["wu_%d_%d" % (k, half)], dma="wgu"))

    xs2 = [xs_main, sb("xsB", [128, 1024], F32, p1b)]
    u2 = [u_sb, sb("u_sbB", [128, 512], F32, p1b)]
    av2 = [avb, sb("avB", [128, 512], F32, p1b)]
    PF = [(S0, "S0"), (OB[0], "OB0")]

    def p1b_block(i, st):
        t0 = i * 128
        par = i % 2
        xs, u_sb, avb = xs2[par], u2[par], av2[par]
        xn, un, an = "xs%d" % par, "u_sb%d" % par, "av%d" % par
        if st == "F":
            load_norm_transpose(i, 0, xs, xn)
            for gi in range(2):
                bank, bn = PF[gi]
                for k in range(8):
                    op("pe", lambda e, k=k, bank=bank, gi=gi: e.matmul(
                        bank[:, :], lhsT=hT[:, 128 * k:128 * k + 128], rhs=w1b[:, k, 512 * gi:512 * gi + 512],
                        start=(k == 0), stop=(k == 7)),
                       reads=["hT%d" % k, "w1b_%d" % k], writes=[bn])
            op("act", lambda e: e.activation(out=u_sb[:], in_=PF[0][0][:, :], func=AF.Copy), reads=["S0"], writes=[un])
            op("dve", lambda e: e.tensor_copy(out=avb[:], in_=PF[1][0][:, :]), reads=["OB0"], writes=[an])
            return
        op("dve", lambda e: e.bn_stats(out=bst[:, 0:6], in_=avb[:]), reads=[an], writes=["bst"])
        op("dve", lambda e: e.bn_aggr(out=mv[:, 0:2], in_=bst[:, 0:6]), reads=["bst"], writes=["mv"])
        op("act", lambda e: e.activation(out=mv[:, 2:3], in_=mv[:, 1:2], func=AF.Ln, bias=epsb[:, 0:1], scale=1.0),
           reads=["mv", "epsb"], writes=["mv2"])
        op("act", lambda e: e.activation(out=mv[:, 3:4], in_=mv[:, 2:3], func=AF.Exp, scale=-0.5),
           reads=["mv2"], writes=["mv3"])
        op("dve", lambda e: e.tensor_scalar(out=avb[:], in0=avb[:], scalar1=mv[:, 0:1], scalar2=mv[:, 3:4],
                                            op0=ALU.subtract, op1=ALU.mult),
           reads=[an, "mv", "mv3"], writes=[an])
        op("dve", lambda e: e.tensor_tensor(out=avb[:], in0=avb[:], in1=lngb[:, 0:512], op=ALU.mult),
           reads=[an, "lngb"], writes=[an])
        op("dve", lambda e: e.tensor_tensor(out=vn[:], in0=avb[:], in1=lngb[:, 512:1024], op=ALU.add),
           reads=[an, "lngb"], writes=["vn"])
        for g in range(4):
            op("pe", lambda e, g=g: e.matmul(MM[2][:, 128 * g:128 * g + 128], lhsT=wsT[:, 128 * g:128 * g + 128],
                                             rhs=vn[:, 128 * g:128 * g + 128], start=True, stop=True),
               reads=["wsT", "vn"], writes=["MM2"])
        for g in range(4):
            op("dve", lambda e, g=g: e.scalar_tensor_tensor(
                out=cata[:, 128 * g:128 * g + 128], in0=MM[2][:, 128 * g:128 * g + 128], scalar=bsT[:, g:g + 1],
                in1=u_sb[:, 128 * g:128 * g + 128], op0=ALU.add, op1=ALU.mult),
               reads=["MM2", "bsT", un], writes=["cata"])
        for k in range(8):
            src = cata[:, 128 * k:128 * k + 128] if k < 4 else outb[:, i, 128 * (k - 4):128 * (k - 4) + 128]
            op("pe", lambda e, k=k, src=src: e.transpose(out=T1[:, 128 * k:128 * k + 128], in_=src, identity=ident[:]),
               reads=["cata", "outb:%d" % i, "ident"], writes=["T1"])
        op("dve", lambda e: e.tensor_copy(out=catT[:, 0:512], in_=T1[:, 0:512]), reads=["T1"], writes=["catTa"])
        op("dve", lambda e: e.tensor_copy(out=catT[:, 512:1024], in_=T1[:, 512:1024]), reads=["T1"], writes=["catTb"])
        for og in range(2):
            for k in range(8):
                op("pe", lambda e, k=k, og=og: e.matmul(
                    MM[og][:, :], lhsT=catT[:, 128 * k:128 * k + 128], rhs=wo[:, k, 512 * og:512 * og + 512],
                    start=(k == 0), stop=(k == 7)),
                   reads=["catTa" if k < 4 else "catTb", "wo_%d" % k], writes=["MM%d" % og])
        op("act", lambda e: e.activation(out=junk[:, 0:512], in_=MM[0][:, :], func=AF.Square, accum_out=ss[:, 2:3]),
           reads=["MM0"], writes=["ssa", "junk"])
        op("act", lambda e: e.activation(out=junk[:, 512:1024], in_=MM[1][:, :], func=AF.Square, accum_out=ss[:, 3:4]),
           reads=["MM1"], writes=["ssb", "junk"])
        op("dve", lambda e: e.tensor_tensor(out=ss[:, 1:2], in0=ss[:, 2:3], in1=ss[:, 3:4], op=ALU.add),
           reads=["ssa", "ssb"], writes=["ss1"])
        rms_rstd(None, 1)
        for og in range(2):
            op("dve", lambda e, og=og: e.scalar_tensor_tensor(
                out=tmpx[:, 512 * og:512 * og + 512], in0=MM[og][:, :], scalar=rstd[:, 1:2],
                in1=gpost[:, 512 * og:512 * og + 512], op0=ALU.mult, op1=ALU.mult),
               reads=["MM%d" % og, "rstd1", "gpost"], writes=["tmpx%d" % og])
        op("dve", lambda e: e.tensor_tensor(out=xs[:], in0=xs[:], in1=tmpx[:], op=ALU.add),
           reads=[xn, "tmpx0", "tmpx1"], writes=[xn])
        op("sp", lambda e, t0=t0: e.dma_start(out=y_d[t0:t0 + 128, :], in_=xs[:]), reads=[xn],
           writes=["y:%d" % i], dma="yst%d" % par)
        if debug:
            op("sp", lambda e, t0=t0: e.dma_start(out=dbg_x1[t0:t0 + 128, :], in_=xs[:]), reads=[xn], dma="dbg3")

    p1b_block(0, "F")
    for i in range(NB):
        la = sc.record(lambda: p1b_block(i, "K"))
        lb = sc.record(lambda: p1b_block(i + 1, "F")) if i + 1 < NB else []
        sc.interleave(la, lb)
        nper = (len(ffn_loads) + NB - 1) // NB
        for _ in range(nper):
            if ffn_loads:
                ffn_loads.pop(0)()
    while ffn_loads:
        ffn_loads.pop(0)()
    sc.regroup("wgu", ["wg_%d_%d" % (k, hf) for k in range(8) for hf in range(2)]
               + ["wu_%d_%d" % (k, hf) for k in range(8) for hf in range(2)])
    sc.barrier()

    check("P1b")
    p2 = None
    aset(P1B_BASE)
    wd = sb("wd", [128, NFF, D_MODEL], BF16, p2)
    aset(32 * 1024)
    gpost = sb("gpost2", [128, 1024], F32, p2)
    op("sp", lambda e: e.dma_start(out=gpost[:], in_=gpost_d[:, 1024:2048]), writes=["gpost"], dma="c3")
    aset(P1B_BASE + NFF * D_MODEL * 2)
    for c in range(NFF):
        op("pool", lambda e, c=c: e.dma_start(out=wd[:, c, :], in_=w_d_d[128 * c:128 * c + 128, :]),
           writes=["wd_%d" % c], dma="wd")
    sc.regroup("wd", ["wd_%d" % c for c in range(NFF)])
    actT = sb("actT", [128, NFF, 512], BF16, p2)
    aset(0)
    x1g = sb("x1g", [128, 4, 1024], F32, p2)
    h2T = sb("h2T", [128, 8, 512], BF16, p2)
    sg = [sb("sg%d" % j, [128, 512], F32, p2) for j in range(2)]
    outx = sb("outx", [128, 1024], F32, p2)
    assert apos[0] <= 32 * 1024
    GA = [MM[0], MM[1]]
    UB = [MM[2], S0]
    FB = [OB[0], OB[1]]
    GAn = ["MM0", "MM1"]
    UBn = ["MM2", "S0"]
    FBn = ["OB0", "OB1"]

    for g in range(NG):
        for b in range(4):
            i = 4 * g + b
            op("sp", lambda e, i=i, b=b: e.dma_start(out=x1g[:, b, :], in_=y_d[128 * i:128 * i + 128, :]),
               reads=["y:%d" % i], writes=["x1g%d" % b], dma="x1g%d" % b)
        for b in range(4):
            op("act", lambda e, b=b: e.activation(out=junk[:, 0:1024], in_=x1g[:, b, :], func=AF.Square,
                                                  accum_out=ss[:, 4 + b:5 + b]),
               reads=["x1g%d" % b], writes=["ss%d" % (4 + b), "junk"])
        for b in range(4):
            rms_rstd(None, 4 + b)
        for b in range(4):
            op("dve", lambda e, b=b: e.tensor_scalar(out=hb[:], in0=x1g[:, b, :], scalar1=rstd[:, 4 + b:5 + b],
                                                     scalar2=None, op0=ALU.mult),
               reads=["x1g%d" % b, "rstd%d" % (4 + b)], writes=["hb"])
            tb = b % 2
            for k in range(8):
                op("pe", lambda e, k=k, tb=tb: e.transpose(out=TB[tb][:, 128 * k:128 * k + 128],
                                                           in_=hb[:, 128 * k:128 * k + 128], identity=ident[:]),
                   reads=["hb", "ident"], writes=["T%d" % tb])
            for k in range(8):
                if True:
                    op("dve", lambda e, k=k, tb=tb, b=b: e.tensor_scalar(
                        out=h2T[:, k, 128 * b:128 * b + 128], in0=TB[tb][:, 128 * k:128 * k + 128],
                        scalar1=gcol[:, 8 + k:9 + k], scalar2=None, op0=ALU.mult),
                       reads=["T%d" % tb, "gcol"], writes=["h2T%d" % k])
                else:
                    op("act", lambda e, k=k, tb=tb, b=b: e.activation(
                        out=h2T[:, k, 128 * b:128 * b + 128], in_=TB[tb][:, 128 * k:128 * k + 128], func=AF.Identity,
                        scale=gcol[:, 8 + k:9 + k]),
                       reads=["T%d" % tb, "gcol"], writes=["h2T%d" % k])
        for c in range(NFF):
            j = c % 2
            for k in range(8):
                op("pe", lambda e, k=k, c=c, j=j: e.matmul(GA[j][:, :], lhsT=wg[:, k, 128 * c:128 * c + 128], rhs=h2T[:, k, :],
                                                           start=(k == 0), stop=(k == 7)),
                   reads=["wg_%d_%d" % (k, 0 if c < 11 else 1), "h2T%d" % k], writes=[GAn[j]])
            for k in range(8):
                op("pe", lambda e, k=k, c=c, j=j: e.matmul(UB[j][:, :], lhsT=wu[:, k, 128 * c:128 * c + 128], rhs=h2T[:, k, :],
                                                           start=(k == 0), stop=(k == 7)),
                   reads=["wu_%d_%d" % (k, 0 if c < 11 else 1), "h2T%d" % k], writes=[UBn[j]])
            op("act", lambda e, j=j: e.activation(out=sg[j][:], in_=GA[j][:, :], func=AF.Silu),
               reads=[GAn[j]], writes=["sg%d" % j])
            op("dve", lambda e, j=j, c=c: e.tensor_tensor(out=actT[:, c, :], in0=sg[j][:], in1=UB[j][:, :], op=ALU.mult),
               reads=["sg%d" % j, UBn[j]], writes=["actT%d" % c])
        for b in range(4):
            i = 4 * g + b
            for og in range(2):
                for c in range(NFF):
                    op("pe", lambda e, c=c, og=og, b=b: e.matmul(
                        FB[og][:, :], lhsT=actT[:, c, 128 * b:128 * b + 128], rhs=wd[:, c, 512 * og:512 * og + 512],
                        start=(c == 0), stop=(c == NFF - 1)),
                       reads=["actT%d" % c, "wd_%d" % c], writes=[FBn[og]])
            op("act", lambda e: e.activation(out=junk[:, 0:512], in_=FB[0][:, :], func=AF.Square, accum_out=ss[:, 2:3]),
               reads=["OB0"], writes=["ssa", "junk"])
            op("act", lambda e: e.activation(out=junk[:, 512:1024], in_=FB[1][:, :], func=AF.Square, accum_out=ss[:, 3:4]),
               reads=["OB1"], writes=["ssb", "junk"])
            op("dve", lambda e: e.tensor_tensor(out=ss[:, 1:2], in0=ss[:, 2:3], in1=ss[:, 3:4], op=ALU.add),
               reads=["ssa", "ssb"], writes=["ss1"])
            rms_rstd(None, 1)
            for og in range(2):
                op("dve", lambda e, og=og: e.scalar_tensor_tensor(
                    out=outx[:, 512 * og:512 * og + 512], in0=FB[og][:, :], scalar=rstd[:, 1:2],
                    in1=gpost[:, 512 * og:512 * og + 512], op0=ALU.mult, op1=ALU.mult),
                   reads=[FBn[og], "rstd1", "gpost"], writes=["outx%d" % og])
            op("dve", lambda e, b=b: e.tensor_tensor(out=outx[:], in0=outx[:], in1=x1g[:, b, :], op=ALU.add),
               reads=["outx0", "outx1", "x1g%d" % b], writes=["outx0", "outx1"])
            op("sp", lambda e, i=i: e.dma_start(out=y_d[128 * i:128 * i + 128, :], in_=outx[:]),
               reads=["outx0", "outx1"], writes=["y:%d" % i], dma="yst")
    return


def _t5_bucket(rel):
    rel = np.asarray(rel, np.int32)
    nb = 16
    ret = (rel > 0).astype(np.int32) * nb
    n = np.abs(rel)
    me = 8
    nf = np.maximum(n, 1).astype(np.float32)
    large = me + (np.log(nf / np.float32(me)) / np.float32(np.log(128 / me)) * np.float32(nb - me)).astype(np.int32)
    large = np.minimum(large, nb - 1)
    return ret + np.where(n < me, n, large)


def host_consts(inp):
    f32 = np.float32
    c = {}
    c["ident"] = np.eye(128, dtype=f32)
    pc = np.arange(128) // 64
    c["vis"] = np.where(pc[None, :] <= pc[:, None], 0.0, NEG).astype(f32)
    c["pw"] = np.broadcast_to((2.0 ** -np.arange(T_BIS + 1)).astype(f32), (128, T_BIS + 1)).copy()
    gcol = np.zeros((128, 16), f32)
    gcol[:, 0:8] = np.asarray(inp["g_pre_mix"], f32).reshape(8, 128).T
    gcol[:, 8:16] = np.asarray(inp["g_pre_ffn"], f32).reshape(8, 128).T
    c["gcol"] = gcol
    gpost = np.zeros((128, 2048), f32)
    gpost[:, 0:1024] = np.asarray(inp["g_post_mix"], f32).reshape(1, 1024)
    gpost[:, 1024:2048] = np.asarray(inp["g_post_ffn"], f32).reshape(1, 1024)
    c["gpost"] = gpost
    ln = np.zeros((128, 1024), f32)
    ln[:, 0:512] = np.asarray(inp["sgu_ln_g"], f32).reshape(1, 512)
    ln[:, 512:1024] = np.asarray(inp["sgu_ln_b"], f32).reshape(1, 512)
    c["lngb"] = ln
    rb = np.asarray(inp["rel_bias"], f32)
    p = np.arange(128)[:, None]
    j = np.arange(256)[None, :]
    bucket = _t5_bucket(j - 128 - p)
    bh = np.zeros((128, 4 * 256 + 4), f32)
    for h in range(4):
        bh[:, 256 * h:256 * h + 256] = rb[bucket, h]
        bh[:, 1024 + h] = rb[15, h]
    c["bh"] = bh
    sw = np.asarray(inp["sgu_w"], f32).reshape(4, 128, 128)
    c["wsT"] = np.ascontiguousarray(sw.transpose(2, 0, 1).reshape(128, 512))
    c["bsT"] = np.ascontiguousarray(np.asarray(inp["sgu_b"], f32).reshape(4, 128).T)
    return c


def make_in_maps(inputs, n_cores, S):
    c = host_consts(inputs)
    shared = dict(c)
    shared["w_in"] = np.ascontiguousarray(np.asarray(inputs["w_in"], np.float32).reshape(D_MODEL, 3144))
    shared["w_o"] = np.ascontiguousarray(np.asarray(inputs["w_o"], np.float32).reshape(D_MODEL, D_MODEL))
    shared["w_gate"] = np.ascontiguousarray(np.asarray(inputs["w_gate"], np.float32).reshape(D_MODEL, D_FF))
    shared["w_up"] = np.ascontiguousarray(np.asarray(inputs["w_up"], np.float32).reshape(D_MODEL, D_FF))
    shared["w_down"] = np.ascontiguousarray(np.asarray(inputs["w_down"], np.float32).reshape(D_FF, D_MODEL))
    x = np.asarray(inputs["x"], np.float32)
    maps = []
    for b in range(n_cores):
        m = dict(shared)
        m["x"] = np.ascontiguousarray(x[b, :S])
        maps.append(m)
    return maps


def kernel(**inputs):
    n = 8
    nc = build(NB=32, debug=False)
    in_maps = make_in_maps(inputs, n, SEQ)
    res = run_bass_kernel_spmd(nc, in_maps, core_ids=list(range(n)))
    out = np.stack([np.asarray(r["y"], np.float32) for r in res.results], axis=0)
    return out
```

```python
import numpy as np
from contextlib import ExitStack
import concourse.bass as bass
import concourse.mybir as mybir
from concourse.bass_utils import run_bass_kernel_spmd
from concourse.alu_op_type import AluOpType as ALU

F32 = mybir.dt.float32
BF16 = mybir.dt.bfloat16
AF = mybir.ActivationFunctionType
AX = mybir.AxisListType

D_MODEL = 1024
SEQ = 4096
D_FF = 2816
NFF = D_FF // 128
TOPK = 256
T_BIS = 14
EPS = 1e-6
NEG = -1.0e30
QSCALE = 128 ** -0.5
ISCALE = (64 ** -0.5) * (8 ** -0.5)


class _Stop(Exception):
    pass


class Sched:
    def __init__(self, nc, es):
        self.nc = nc
        self.es = es
        self.eng = {"pe": nc.tensor, "act": nc.scalar, "dve": nc.vector, "pool": nc.gpsimd, "sp": nc.sync}
        self.semh = {}
        self.cnt = {}
        for e in self.eng:
            self.semh[e] = es.enter_context(nc.semaphore("sem_" + e))
            self.cnt[e] = 0
        self.seen = {e: {} for e in self.eng}
        self.lastw = {}
        self.readers = {}
        self.nwait = 0
        self.nops = 0
        self.stop_ops = None
        self.log = None
        self.rec = None

    def _dma_key(self, slot):
        key = "dma:" + slot
        if key not in self.semh:
            self.semh[key] = self.es.enter_context(self.nc.semaphore("sd_" + slot))
            self.cnt[key] = 0
        return key

    PSUM_RES = frozenset(["MM0", "MM1", "MM2", "S0", "OB0", "OB1", "T0", "T1"])

    def record(self, f):
        saved = self.rec
        self.rec = []
        f()
        out = self.rec
        self.rec = saved
        return out

    def interleave(self, la, lb):
        na, nb = len(la), len(lb)
        ia = ib = 0
        import os
        if os.environ.get("NOINTER"):
            for a in la: self.op(*a)
            for b in lb: self.op(*b)
            return
        while ia < na or ib < nb:
            if ib >= nb or (ia < na and ia * nb <= ib * na):
                self.op(*la[ia])
                ia += 1
            else:
                self.op(*lb[ib])
                ib += 1

    def op(self, eng, fn, reads=(), writes=(), dma=None):
        if self.rec is not None:
            self.rec.append((eng, fn, tuple(reads), tuple(writes), dma))
            return None
        pr = [r for r in reads if r in self.PSUM_RES]
        if pr:
            writes = list(writes) + pr
        deps = {}

        def add(d, raw):
            if d is None:
                return
            key, val, deng = d
            if deng == eng and not key.startswith("dma:"):
                if eng in ("pe", "sp"):
                    return
            if deps.get(key, 0) < val:
                deps[key] = val

        for r in reads:
            add(self.lastw.get(r), True)
        for w in writes:
            add(self.lastw.get(w), False)
            for key, (val, deng) in self.readers.get(w, {}).items():
                add((key, val, deng), False)
        E = self.eng[eng]
        for key, val in deps.items():
            if self.seen[eng].get(key, 0) < val:
                E.wait_ge(self.semh[key], val)
                self.seen[eng][key] = val
                self.nwait += 1
        if self.stop_ops is not None and self.nops >= self.stop_ops:
            raise _Stop()
        inst = fn(E)
        if self.log is not None:
            import sys as _s
            self.log.append((self.nops, eng, _s._getframe(1).f_lineno, tuple(reads), tuple(writes)))
        self.nops += 1
        if dma is not None:
            key = self._dma_key(dma)
            self.cnt[key] += 16
            inst.then_inc(self.semh[key], 16)
        else:
            key = eng
            self.cnt[key] += 1
            inst.then_inc(self.semh[key], 1)
        val = self.cnt[key]
        for r in reads:
            d = self.readers.setdefault(r, {})
            if d.get(key, (0, None))[0] < val:
                d[key] = (val, eng)
        for w in writes:
            self.lastw[w] = (key, val, eng)
            self.readers[w] = {}
        return inst

    def regroup(self, slot, names):
        key = "dma:" + slot
        for n in names:
            k0, v0, e0 = self.lastw[n]
            self.lastw[n] = (key, self.cnt[key], e0)

    def barrier(self):
        for e in self.eng:
            E = self.eng[e]
            for key, val in self.cnt.items():
                if val > 0 and key != e and self.seen[e].get(key, 0) < val:
                    E.wait_ge(self.semh[key], val)
                    self.seen[e][key] = val

    def final_wait(self, eng="sp"):
        E = self.eng[eng]
        for key, val in self.cnt.items():
            if val > 0 and key != eng and self.seen[eng].get(key, 0) < val:
                E.wait_ge(self.semh[key], val)
                self.seen[eng][key] = val


def skew_emit(items, nstage):
    n = len(items)
    for step in range(n + nstage - 1):
        for s in range(nstage):
            j = step - s
            if 0 <= j < n:
                items[j][s]()


def build(NB=32, debug=False, stop=None, stop_ops=None):
    nc = bass.Bass("TRN2", target_bir_lowering=False)
    es = ExitStack()
    sc = Sched(nc, es)
    sc.stop_ops = stop_ops
    try:
        _build_body(nc, es, sc, NB, debug, stop)
    except _Stop:
        pass
    sc.final_wait("sp")
    es.close()
    nc._sched_stats = (sc.nops, sc.nwait)
    return nc


def _build_body(nc, es, sc, NB, debug, stop):
    def check(tag):
        if stop == tag:
            raise _Stop()

    S = NB * 128
    NG = NB // 4
    dt = nc.dram_tensor
    x_d = dt("x", [S, D_MODEL], F32, kind="ExternalInput").ap()
    w_in_d = dt("w_in", [D_MODEL, 3144], F32, kind="ExternalInput").ap()
    w_o_d = dt("w_o", [D_MODEL, D_MODEL], F32, kind="ExternalInput").ap()
    w_g_d = dt("w_gate", [D_MODEL, D_FF], F32, kind="ExternalInput").ap()
    w_u_d = dt("w_up", [D_MODEL, D_FF], F32, kind="ExternalInput").ap()
    w_d_d = dt("w_down", [D_FF, D_MODEL], F32, kind="ExternalInput").ap()
    ident_d = dt("ident", [128, 128], F32, kind="ExternalInput").ap()
    vis_d = dt("vis", [128, 128], F32, kind="ExternalInput").ap()
    pw_d = dt("pw", [128, T_BIS + 1], F32, kind="ExternalInput").ap()
    gcol_d = dt("gcol", [128, 16], F32, kind="ExternalInput").ap()
    gpost_d = dt("gpost", [128, 2048], F32, kind="ExternalInput").ap()
    ln_d = dt("lngb", [128, 1024], F32, kind="ExternalInput").ap()
    bh_d = dt("bh", [128, 4 * 256 + 4], F32, kind="ExternalInput").ap()
    wsT_d = dt("wsT", [128, 512], F32, kind="ExternalInput").ap()
    bs_d = dt("bsT", [128, 4], F32, kind="ExternalInput").ap()
    y_d = dt("y", [S, D_MODEL], F32, kind="ExternalOutput").ap()
    if debug:
        dbg_ob = dt("dbg_ob", [S, 512], BF16, kind="ExternalOutput").ap()
        dbg_sc = dt("dbg_sc", [128, 4096], F32, kind="ExternalOutput").ap()
        dbg_thr = dt("dbg_thr", [128, NB], F32, kind="ExternalOutput").ap()
        dbg_x1 = dt("dbg_x1", [S, D_MODEL], F32, kind="ExternalOutput").ap()

    op = sc.op

    def sb(name, shape, dtype, stack=es):
        return stack.enter_context(nc.sbuf_tensor("s_" + name, shape, dtype))

    def ps(name, shape, dtype, stack=es):
        return stack.enter_context(nc.psum_tensor("p_" + name, shape, dtype))

    ARENA_BYTES = 198 * 1024
    arena = es.enter_context(nc.sbuf_tensor("s_arena", [128, ARENA_BYTES // 2], BF16))
    apos = [0]

    def aset(off_bytes):
        apos[0] = off_bytes

    def av(name, shape, dtype, stack=None):
        esz = 4 if dtype == F32 else 2
        nel = 1
        for d in shape[1:]:
            nel *= d
        nbytes = (nel * esz + 63) // 64 * 64
        off = apos[0]
        assert off % 64 == 0 and off + nbytes <= ARENA_BYTES, (name, off, nbytes)
        apos[0] = off + nbytes
        v = arena[:, off // 2:off // 2 + nel * esz // 2]
        if dtype == F32:
            v = v.bitcast(F32)
        if len(shape) == 3:
            v = v.rearrange("p (a b) -> p a b", a=shape[1])
        elif len(shape) == 4:
            v = v.rearrange("p (a b c) -> p a b c", a=shape[1], b=shape[2])
        return v

    T0 = ps("T0", [128, 1024], BF16)
    T1 = ps("T1", [128, 1024], BF16)
    MM = [ps("MM%d" % i, [128, 512], F32) for i in range(3)]
    S0 = ps("S0", [128, 512], F32)
    OB = [ps("OB%d" % i, [128, 512], F32) for i in range(2)]
    TB = [T0, T1]
    LGB = [(MM[2][:, :], "MM2"), (T1[:, :].bitcast(F32), "T1")]

    ident = sb("ident", [128, 128], BF16)
    gcol = sb("gcol", [128, 16], F32)
    junk = sb("junk", [128, 1024], BF16)
    ss = sb("ss", [128, 8], F32)
    rstd = sb("rstd", [128, 8], F32)
    hb = sb("hb", [128, 1024], BF16)
    epsb = sb("epsb", [128, 2], F32)
    aset(0)
    outb = av("outb", [128, 32, 512], BF16)
    xs_main = av("xs", [128, 1024], F32)
    hT = av("hT", [128, 1024], BF16)
    P1A_BASE = 38 * 1024
    aset(P1A_BASE)
    sb = av
    p1a = None
    vis = sb("vis", [128, 128], F32, p1a)
    pw = sb("pw", [128, T_BIS + 1], F32, p1a)
    bh = sb("bh", [128, 4 * 256 + 4], F32, p1a)

    op("pool", lambda e: e.dma_start(out=ident[:], in_=ident_d[:, :]), writes=["ident"], dma="c_ident")
    op("sp", lambda e: e.dma_start(out=vis[:], in_=vis_d[:, :]), writes=["vis"], dma="c0")
    op("sp", lambda e: e.dma_start(out=pw[:], in_=pw_d[:, :]), writes=["pw"], dma="c1")
    op("sp", lambda e: e.dma_start(out=gcol[:], in_=gcol_d[:, :]), writes=["gcol"], dma="c2")
    op("sp", lambda e: e.dma_start(out=bh[:], in_=bh_d[:, :]), writes=["bh"], dma="c4")

    def rms_rstd(src_ops, col):
        op("act", lambda e: e.activation(out=rstd[:, col:col + 1], in_=ss[:, col:col + 1], func=AF.Ln,
                                         bias=epsb[:, 0:1], scale=1.0 / 1024),
           reads=["ss%d" % col, "epsb"], writes=["rstd%d" % col])
        op("act", lambda e: e.activation(out=rstd[:, col:col + 1], in_=rstd[:, col:col + 1], func=AF.Exp,
                                         scale=-0.5),
           reads=["rstd%d" % col], writes=["rstd%d" % col])

    op("dve", lambda e: e.memset(epsb[:, 0:1], EPS), writes=["epsb"])
    check("consts")

    def load_norm_transpose(i, gc0, xs=None, xn="xs"):
        if xs is None:
            xs = xs_main
        t0 = i * 128
        op("sp", lambda e: e.dma_start(out=xs[:], in_=x_d[t0:t0 + 128, :]), writes=[xn], dma=xn)
        op("act", lambda e: e.activation(out=junk[:, 0:1024], in_=xs[:], func=AF.Square, accum_out=ss[:, 0:1]),
           reads=[xn], writes=["ss0", "junk"])
        rms_rstd(None, 0)
        op("dve", lambda e: e.tensor_scalar(out=hb[:], in0=xs[:], scalar1=rstd[:, 0:1], scalar2=None, op0=ALU.mult),
           reads=[xn, "rstd0"], writes=["hb"])
        for k in range(8):
            op("pe", lambda e, k=k: e.transpose(out=T0[:, 128 * k:128 * k + 128], in_=hb[:, 128 * k:128 * k + 128],
                                                identity=ident[:]),
               reads=["hb", "ident"], writes=["T0"])
        for k in range(8):
            if True:
                op("dve", lambda e, k=k: e.tensor_scalar(out=hT[:, 128 * k:128 * k + 128], in0=T0[:, 128 * k:128 * k + 128],
                                                         scalar1=gcol[:, gc0 + k:gc0 + k + 1], scalar2=None, op0=ALU.mult),
                   reads=["T0", "gcol"], writes=["hT%d" % k])
            else:
                op("act", lambda e, k=k: e.activation(out=hT[:, 128 * k:128 * k + 128], in_=T0[:, 128 * k:128 * k + 128],
                                                      func=AF.Identity, scale=gcol[:, gc0 + k:gc0 + k + 1]),
                   reads=["T0", "gcol"], writes=["hT%d" % k])

    w1 = sb("w1", [128, 8, 2120], BF16, p1a)
    kT = sb("kT", [128, 4, S], BF16, p1a)
    vc = sb("vc", [128, NB, 4, 130], BF16, p1a)
    ikT = sb("ikT", [128, S], BF16, p1a)
    score = sb("score", [128, S], F32, p1a)
    sel = sb("sel", [128, S], BF16, p1a)
    q_sb = sb("q_sb", [128, 512], BF16, p1a)
    k_sb = sb("k_sb", [128, 512], BF16, p1a)
    iq_sb = sb("iq_sb", [128, 512], BF16, p1a)
    ik2 = sb("ik2", [128, 128], BF16, p1a)
    qT2 = [sb("qT%d" % j, [128, 512], BF16, p1a) for j in range(2)]
    iqT2 = [sb("iqT%d" % j, [128, 512], BF16, p1a) for j in range(2)]
    Dg2 = [sb("Dg%d" % j, [128, 8, 128], BF16, p1a) for j in range(2)]
    iwc2 = [sb("iwc%d" % j, [128, 8], F32, p1a) for j in range(2)]
    junkd = sb("junkd", [128, 512], BF16, p1a)
    rbuf = [sb("r%d" % j, [128, 512], BF16, p1a) for j in range(3)]
    Eb = [sb("E%d" % j, [128, 512], BF16, p1a) for j in range(2)]
    Pb = [sb("P%d" % j, [128, 512], BF16, p1a) for j in range(2)]
    PTb = [sb("PT%d" % j, [128, 512], BF16, p1a) for j in range(2)]
    tmpb = sb("tmpb", [128, 256], F32, p1a)
    amaxp = sb("amaxp", [128, 8], F32, p1a)
    aminp = sb("aminp", [128, 8], F32, p1a)
    bis = sb("bis", [128, 8], F32, p1a)
    D2 = sb("D2", [128, T_BIS + 1], F32, p1a)
    thr_all = sb("thr_all", [128, NB], F32, p1a)

    for k in range(8):
        op("pool", lambda e, k=k: e.dma_start(out=w1[:, k, :], in_=w_in_d[128 * k:128 * k + 128, 1024:3144],
                                              max_dma_last_dim=4096),
           writes=["w1_%d" % k], dma="w1")
    sc.regroup("w1", ["w1_%d" % k for k in range(8)])
    op("dve", lambda e: e.memset(vc[:, :, :, 128:130], 1.0), writes=["vc_ones"])

    dots_rot = [0]
    lg_rot = [0]
    e_rot = [0]
    tb_rot = [0]

    def p1a_block(i, st):
        t0 = i * 128
        L = t0 + 128
        par = i % 2
        nkt = (L + 511) // 512
        qT, iqT, Dg, iwc = qT2[par], iqT2[par], Dg2[par], iwc2[par]
        nqT, niqT, niwc = "qT_%d" % par, "iqT_%d" % par, "iwc_%d" % par
        if st == "A":
            load_norm_transpose(i, 0)
            groups = [("q", 0, 512), ("k", 512, 512), ("v", 1024, 512), ("iq", 1536, 512), ("ik", 2048, 72)]
            for gi, (nm, c0, w) in enumerate(groups):
                bank = MM[gi % 3]
                bname = "MM%d" % (gi % 3)
                for k in range(8):
                    op("pe", lambda e, k=k, bank=bank, c0=c0, w=w: e.matmul(
                        bank[:, 0:w], lhsT=hT[:, 128 * k:128 * k + 128], rhs=w1[:, k, c0:c0 + w],
                        start=(k == 0), stop=(k == 7)),
                       reads=["hT%d" % k, "w1_%d" % k], writes=[bname])
                if nm == "q":
                    op("act", lambda e, bank=bank: e.activation(out=q_sb[:], in_=bank[:, :], func=AF.Copy),
                       reads=[bname], writes=["q_sb"])
                elif nm == "k":
                    op("dve", lambda e, bank=bank: e.tensor_copy(out=k_sb[:], in_=bank[:, :]),
                       reads=[bname], writes=["k_sb"])
                elif nm == "v":
                    op("act", lambda e, bank=bank, i=i: e.activation(
                        out=vc[:, i, :, 0:128], in_=bank[:, :].rearrange("p (h d) -> p h d", h=4), func=AF.Copy),
                       reads=[bname], writes=["vc:%d" % i])
                elif nm == "iq":
                    op("dve", lambda e, bank=bank: e.tensor_copy(out=iq_sb[:], in_=bank[:, :]),
                       reads=[bname], writes=["iq_sb"])
                else:
                    op("act", lambda e, bank=bank: e.activation(out=ik2[:, 0:64], in_=bank[:, 0:64], func=AF.Copy),
                       reads=[bname], writes=["ik2a"])
                    op("dve", lambda e, bank=bank: e.tensor_copy(out=ik2[:, 64:128], in_=bank[:, 0:64]),
                       reads=[bname], writes=["ik2b"])
                    op("dve", lambda e, bank=bank: e.tensor_scalar(out=iwc[:], in0=bank[:, 64:72], scalar1=ISCALE,
                                                                   scalar2=None, op0=ALU.mult),
                       reads=[bname], writes=[niwc])
            for h in range(4):
                op("pe", lambda e, h=h: e.transpose(out=T1[:, 128 * h:128 * h + 128], in_=q_sb[:, 128 * h:128 * h + 128],
                                                    identity=ident[:]),
                   reads=["q_sb", "ident"], writes=["T1"])
            for h in range(4):
                op("pe", lambda e, h=h: e.transpose(out=T1[:, 512 + 128 * h:512 + 128 * h + 128],
                                                    in_=k_sb[:, 128 * h:128 * h + 128], identity=ident[:]),
                   reads=["k_sb", "ident"], writes=["T1"])
            op("dve", lambda e: e.tensor_copy(out=qT[:], in_=T1[:, 0:512]), reads=["T1"], writes=[nqT])
            op("dve", lambda e, t0=t0: e.tensor_copy(out=kT[:, :, t0:t0 + 128],
                                                     in_=T1[:, 512:1024].rearrange("p (h t) -> p h t", h=4)),
               reads=["T1"], writes=["kT:%d" % i])
            for h in range(4):
                op("pe", lambda e, h=h: e.transpose(out=T0[:, 128 * h:128 * h + 128], in_=iq_sb[:, 128 * h:128 * h + 128],
                                                    identity=ident[:]),
                   reads=["iq_sb", "ident"], writes=["T0"])
            op("pe", lambda e: e.transpose(out=T0[:, 512:640], in_=ik2[:, :], identity=ident[:]),
               reads=["ik2a", "ik2b", "ident"], writes=["T0"])
            op("dve", lambda e: e.tensor_copy(out=iqT[:], in_=T0[:, 0:512]), reads=["T0"], writes=[niqT])
            op("dve", lambda e, t0=t0: e.tensor_copy(out=ikT[:, t0:t0 + 128], in_=T0[:, 512:640]),
               reads=["T0"], writes=["ikT:%d" % i])
            for h in range(8):
                op("dve", lambda e, h=h: e.tensor_scalar(out=Dg[:, h, :], in0=ident[:], scalar1=iwc[:, h:h + 1],
                                                          scalar2=1.0, op0=ALU.mult, op1=ALU.mult),
                   reads=["ident", niwc], writes=["Dg%d_%d" % (par, h)])

        if st == "I":
            pairs = [(kt, h) for kt in range(nkt) for h in range(8)]

            def emit_dots(n):
                kt, h = pairs[n]
                N = min(512, L - 512 * kt)
                b = dots_rot[0] % 2
                dots_rot[0] += 1
                pb = 64 * (h % 2)
                cb = 128 * (h // 2)
                blks = ["ikT:%d" % bb for bb in range(4 * kt, min(4 * kt + 4, i + 1))]
                op("pe", lambda e: e.matmul(MM[b][:, 0:N], lhsT=iqT[pb:pb + 64, cb:cb + 128],
                                            rhs=ikT[pb:pb + 64, 512 * kt:512 * kt + N], start=True, stop=True),
                   reads=[niqT] + blks, writes=["MM%d" % b])
                return b

            dbank = {}
            for n in range(min(2, len(pairs))):
                dbank[n] = emit_dots(n)
            for n, (kt, h) in enumerate(pairs):
                N = min(512, L - 512 * kt)
                b = dbank[n]
                rj = n % 3
                if True:
                    op("act", lambda e, b=b, rj=rj, N=N: e.activation(out=rbuf[rj][:, 0:N], in_=MM[b][:, 0:N], func=AF.Relu),
                       reads=["MM%d" % b], writes=["r%d" % rj])
                else:
                    op("dve", lambda e, b=b, rj=rj, N=N: e.tensor_scalar(out=rbuf[rj][:, 0:N], in0=MM[b][:, 0:N], scalar1=0.0,
                                                                         scalar2=None, op0=ALU.max),
                       reads=["MM%d" % b], writes=["r%d" % rj])
                op("pe", lambda e, rj=rj, N=N, h=h: e.matmul(S0[:, 0:N], lhsT=Dg[:, h, :], rhs=rbuf[rj][:, 0:N],
                                                             start=(h == 0), stop=(h == 7)),
                   reads=["r%d" % rj, "Dg%d_%d" % (par, h)], writes=["S0"])
                if n + 2 < len(pairs):
                    dbank[n + 2] = emit_dots(n + 2)
                if h == 7:
                    op("act", lambda e, kt=kt, N=N: e.activation(out=score[:, 512 * kt:512 * kt + N], in_=S0[:, 0:N],
                                                                 func=AF.Copy),
                       reads=["S0"], writes=["score"])
                    op("dve", lambda e, kt=kt, N=N: e.tensor_scalar(
                        out=junkd[:, 0:N], in0=score[:, 512 * kt:512 * kt + N], scalar1=-3.0e38, scalar2=None,
                        op0=ALU.max, op1=ALU.max, accum_out=amaxp[:, kt:kt + 1]),
                       reads=["score"], writes=["amaxp", "junkd"])
                    op("dve", lambda e, kt=kt, N=N: e.tensor_scalar(
                        out=junkd[:, 0:N], in0=score[:, 512 * kt:512 * kt + N], scalar1=3.0e38, scalar2=None,
                        op0=ALU.min, op1=ALU.min, accum_out=aminp[:, kt:kt + 1]),
                       reads=["score"], writes=["aminp", "junkd"])
            op("dve", lambda e, L=L: e.tensor_tensor(out=score[:, L - 128:L], in0=score[:, L - 128:L], in1=vis[:],
                                                      op=ALU.add),
               reads=["score", "vis"], writes=["score"])

        if st == "B":
            A_, MID, CNT, PM, THR = (bis[:, j:j + 1] for j in range(5))
            if L > TOPK:
                op("dve", lambda e: e.tensor_scalar(out=amaxp[:, 0:nkt], in0=amaxp[:, 0:nkt], scalar1=-3.0e38, scalar2=None,
                                                    op0=ALU.max, op1=ALU.max, accum_out=bis[:, 6:7]),
                   reads=["amaxp"], writes=["amaxp", "bis6"])
                op("dve", lambda e: e.tensor_scalar(out=aminp[:, 0:nkt], in0=aminp[:, 0:nkt], scalar1=3.0e38, scalar2=None,
                                                    op0=ALU.min, op1=ALU.min, accum_out=bis[:, 7:8]),
                   reads=["aminp"], writes=["aminp", "bis7"])
                op("dve", lambda e: e.scalar_tensor_tensor(out=A_, in0=bis[:, 7:8], scalar=-1.0, in1=bis[:, 6:7],
                                                           op0=ALU.mult, op1=ALU.max),
                   reads=["bis6", "bis7"], writes=["bisA"])
                op("dve", lambda e: e.tensor_scalar(out=D2[:], in0=pw[:], scalar1=A_, scalar2=None, op0=ALU.mult),
                   reads=["bisA", "pw"], writes=["D2"])
                op("dve", lambda e: e.memset(MID, 0.0), writes=["mid"])
                Ld = L if L < 1024 else ((L * 9 // 20 + 127) // 128) * 128
                nA = L - Ld
                for k in range(T_BIS):
                    op("dve", lambda e: e.tensor_scalar(out=sel[:, 0:Ld], in0=score[:, 0:Ld], scalar1=MID, scalar2=None,
                                                        op0=ALU.is_gt, op1=ALU.add, accum_out=CNT),
                       reads=["score", "mid"], writes=["cnt", "sel"])
                    if nA > 0:
                        op("act", lambda e: e.activation(out=sel[:, Ld:L], in_=score[:, Ld:L], func=AF.Sign,
                                                         bias=MID, scale=-1.0, accum_out=bis[:, 6:7]),
                           reads=["score", "mid"], writes=["bis6", "selB"])
                        op("dve", lambda e: e.scalar_tensor_tensor(out=CNT, in0=bis[:, 6:7], scalar=-0.5, in1=CNT,
                                                                   op0=ALU.mult, op1=ALU.add),
                           reads=["bis6", "cnt"], writes=["cnt"])
                    op("dve", lambda e: e.tensor_scalar(out=PM, in0=CNT, scalar1=TOPK - 0.5 - nA / 2.0, scalar2=0.5,
                                                        op0=ALU.is_gt, op1=ALU.subtract),
                       reads=["cnt"], writes=["pm"])
                    op("dve", lambda e, k=k: e.scalar_tensor_tensor(out=MID, in0=PM, scalar=D2[:, k:k + 1], in1=MID,
                                                                    op0=ALU.mult, op1=ALU.add),
                       reads=["pm", "D2", "mid"], writes=["mid"])
                op("dve", lambda e: e.tensor_tensor(out=THR, in0=MID, in1=D2[:, T_BIS:T_BIS + 1], op=ALU.subtract),
                   reads=["mid", "D2"], writes=["thr"])
            else:
                op("dve", lambda e: e.memset(THR, -1.0e29), writes=["thr"])
            if debug:
                op("dve", lambda e, i=i: e.tensor_copy(out=thr_all[:, i:i + 1], in_=THR), reads=["thr"], writes=["thr_all"])
            op("dve", lambda e, L=L: e.tensor_scalar(out=sel[:, 0:L], in0=score[:, 0:L], scalar1=THR, scalar2=None,
                                                     op0=ALU.is_gt),
               reads=["score", "thr"], writes=["sel", "selB"])

        if st == "C":
            ntile = (L + 511) // 512
            bw = min(256, L)
            items = []
            for h in range(4):
                for j in range(ntile - 1, -1, -1):
                    c1 = L - 512 * j
                    c0 = max(0, c1 - 512)
                    N = c1 - c0
                    first = (j == ntile - 1)
                    last = (j == 0)
                    st = {}

                    def s1(h=h, c0=c0, c1=c1, N=N, j=j, st=st):
                        LG, lgn = LGB[lg_rot[0] % 2]
                        lg_rot[0] += 1
                        ej = e_rot[0] % 2
                        e_rot[0] += 1
                        st["ej"] = ej
                        blks = ["kT:%d" % bb for bb in range(c0 // 128, c1 // 128)]
                        op("pe", lambda e: e.matmul(LG[:, 0:N], lhsT=qT[:, 128 * h:128 * h + 128], rhs=kT[:, h, c0:c1],
                                                    start=True, stop=True),
                           reads=[nqT] + blks, writes=[lgn])
                        nb_ = bw if j == 0 else 0
                        nf = N - nb_
                        if nf > 0:
                            op("act", lambda e: e.activation(out=Eb[ej][:, 0:nf], in_=LG[:, 0:nf], func=AF.Exp,
                                                             bias=bh[:, 1024 + h:1025 + h], scale=QSCALE),
                               reads=[lgn, "bh"], writes=["E%d" % ej])
                        if nb_ > 0:
                            op("dve", lambda e: e.scalar_tensor_tensor(
                                out=tmpb[:, 0:nb_], in0=LG[:, nf:N], scalar=QSCALE,
                                in1=bh[:, 256 * h + 256 - nb_:256 * h + 256], op0=ALU.mult, op1=ALU.add),
                               reads=[lgn, "bh"], writes=["tmpb"])
                            op("act", lambda e: e.activation(out=Eb[ej][:, nf:N], in_=tmpb[:, 0:nb_], func=AF.Exp),
                               reads=["tmpb"], writes=["E%d" % ej])
                        op("dve", lambda e: e.tensor_tensor(out=Pb[ej][:, 0:N], in0=Eb[ej][:, 0:N], in1=sel[:, c0:c1],
                                                            op=ALU.mult),
                           reads=["E%d" % ej, "sel", "selB"], writes=["P%d" % ej])

                    def s2(N=N, st=st):
                        ej = st["ej"]
                        tb = 0
                        pj = tb_rot[0] % 2
                        tb_rot[0] += 1
                        st["pj"] = pj
                        st["tb"] = tb
                        for m in range(N // 128):
                            op("pe", lambda e, m=m: e.transpose(out=TB[tb][:, 128 * m:128 * m + 128],
                                                                in_=Pb[ej][:, 128 * m:128 * m + 128], identity=ident[:]),
                               reads=["P%d" % ej, "ident"], writes=["T%d" % tb])
                        eng = "dve"
                        if eng == "act":
                            op("act", lambda e: e.activation(out=PTb[pj][:, 0:N], in_=TB[tb][:, 0:N], func=AF.Copy),
                               reads=["T%d" % tb], writes=["PT%d" % pj])
                        else:
                            op("dve", lambda e: e.tensor_copy(out=PTb[pj][:, 0:N], in_=TB[tb][:, 0:N]),
                               reads=["T%d" % tb], writes=["PT%d" % pj])

                    def s3(h=h, c0=c0, N=N, first=first, last=last, st=st, i=i):
                        pj = st["pj"]
                        ob = OB[h // 2]
                        off = 130 * (h % 2)
                        nm = N // 128
                        for m in range(nm):
                            blk = c0 // 128 + m
                            op("pe", lambda e, m=m, blk=blk: e.matmul(
                                ob[:, off:off + 130], lhsT=PTb[pj][:, 128 * m:128 * m + 128], rhs=vc[:, blk, h, :],
                                start=(first and m == 0), stop=(last and m == nm - 1)),
                               reads=["PT%d" % pj, "vc:%d" % blk, "vc_ones"], writes=["OB%d" % (h // 2)])
                        if last:
                            RS = bis[:, 5:6]
                            op("dve", lambda e: e.reciprocal(out=RS, in_=ob[:, off + 128:off + 129]),
                               reads=["OB%d" % (h // 2)], writes=["rs"])
                            op("dve", lambda e: e.tensor_scalar(out=outb[:, i, 128 * h:128 * h + 128], in0=ob[:, off:off + 128],
                                                                scalar1=RS, scalar2=None, op0=ALU.mult),
                               reads=["OB%d" % (h // 2), "rs"], writes=["outb:%d" % i])

                    items.append([s1, s2, s3])
            skew_emit(items, 3)

    rec = sc.record
    p1a_block(0, 'A')
    p1a_block(0, 'I')
    for i in range(NB):
        la = rec(lambda: p1a_block(i, 'B'))
        lb = rec(lambda: p1a_block(i + 1, 'A')) if i + 1 < NB else []
        sc.interleave(la, lb)
        la = rec(lambda: p1a_block(i, 'C'))
        lb = rec(lambda: p1a_block(i + 1, 'I')) if i + 1 < NB else []
        sc.interleave(la, lb)
        check("C%d" % i)

    if debug:
        op("sp", lambda e: e.dma_start(out=dbg_sc[:, 0:S], in_=score[:, 0:S]), reads=["score"], dma="dbg0")
        op("sp", lambda e: e.dma_start(out=dbg_thr[:, :], in_=thr_all[:]), reads=["thr_all"], dma="dbg1")
        for i in range(NB):
            op("sp", lambda e, i=i: e.dma_start(out=dbg_ob[128 * i:128 * i + 128, :], in_=outb[:, i, :]),
               reads=["outb:%d" % i], dma="dbg2")
    sc.barrier()

    check("P1a")
    aset(P1A_BASE)
    wg = sb("wg", [128, 8, D_FF], BF16)
    wu = sb("wu", [128, 8, D_FF], BF16)
    P1B_BASE = apos[0]
    p1b = None
    w1b = sb("w1b", [128, 8, 1024], BF16, p1b)
    wo = sb("wo", [128, 8, 1024], BF16, p1b)
    lngb = sb("lngb", [128, 1024], F32, p1b)
    wsT = sb("wsT", [128, 512], BF16, p1b)
    bsT = sb("bsT", [128, 4], F32, p1b)
    u_sb = sb("u_sb", [128, 512], F32, p1b)
    avb = sb("av", [128, 512], F32, p1b)
    vn = sb("vn", [128, 512], BF16, p1b)
    cata = sb("cata", [128, 512], BF16, p1b)
    catT = sb("catT", [128, 1024], BF16, p1b)
    tmpx = sb("tmpx", [128, 1024], F32, p1b)
    bst = sb("bst", [128, 8], F32, p1b)
    mv = sb("mv", [128, 8], F32, p1b)
    gpost = sb("gpost1", [128, 1024], F32, p1b)
    op("sp", lambda e: e.dma_start(out=gpost[:], in_=gpost_d[:, 0:1024]), writes=["gpost"], dma="c3")

    for k in range(8):
        op("pool", lambda e, k=k: e.dma_start(out=w1b[:, k, :], in_=w_in_d[128 * k:128 * k + 128, 0:1024]),
           writes=["w1b_%d" % k], dma="wp1b")
    for k in range(8):
        op("pool", lambda e, k=k: e.dma_start(out=wo[:, k, :], in_=w_o_d[128 * k:128 * k + 128, :]),
           writes=["wo_%d" % k], dma="wp1b")
    sc.regroup("wp1b", ["w1b_%d" % k for k in range(8)] + ["wo_%d" % k for k in range(8)])
    op("sp", lambda e: e.dma_start(out=lngb[:], in_=ln_d[:, :]), writes=["lngb"], dma="c0")
    op("pool", lambda e: e.dma_start(out=wsT[:], in_=wsT_d[:, :]), writes=["wsT"], dma="c_ident")
    op("sp", lambda e: e.dma_start(out=bsT[:], in_=bs_d[:, :]), writes=["bsT"], dma="c1")
    for g in range(4):
        op("dve", lambda e, g=g: e.memset(wsT[64:128, 128 * g:128 * g + 64], 0.0), reads=["wsT"], writes=["wsT"])

    ffn_loads = []
    for k in range(8):
        for half in range(2):
            c0 = half * 1408
            ffn_loads.append(lambda k=k, c0=c0, half=half: op(
                "pool", lambda e: e.dma_start(out=wg[:, k, c0:c0 + 1408], in_=w_g_d[128 * k:128 * k + 128, c0:c0 + 1408]),
                writes=["wg_%d_%d" % (k, half)], dma="wgu"))
            ffn_loads.append(lambda k=k, c0=c0, half=half: op(
                "pool", lambda e: e.dma_start(out=wu[:, k, c0:c0 + 1408], in_=w_u_d[128 * k:128 * k + 128, c0:c0 + 1408]),
                writes=["wu_%d_%d" % (k, half)], dma="wgu"))

    xs2 = [xs_main, sb("xsB", [128, 1024], F32, p1b)]
    u2 = [u_sb, sb("u_sbB", [128, 512], F32, p1b)]
    av2 = [avb, sb("avB", [128, 512], F32, p1b)]
    PF = [(S0, "S0"), (OB[0], "OB0")]

    def p1b_block(i, st):
        t0 = i * 128
        par = i % 2
        xs, u_sb, avb = xs2[par], u2[par], av2[par]
        xn, un, an = "xs%d" % par, "u_sb%d" % par, "av%d" % par
        if st == "F":
            load_norm_transpose(i, 0, xs, xn)
            for gi in range(2):
                bank, bn = PF[gi]
                for k in range(8):
                    op("pe", lambda e, k=k, bank=bank, gi=gi: e.matmul(
                        bank[:, :], lhsT=hT[:, 128 * k:128 * k + 128], rhs=w1b[:, k, 512 * gi:512 * gi + 512],
                        start=(k == 0), stop=(k == 7)),
                       reads=["hT%d" % k, "w1b_%d" % k], writes=[bn])
            op("act", lambda e: e.activation(out=u_sb[:], in_=PF[0][0][:, :], func=AF.Copy), reads=["S0"], writes=[un])
            op("dve", lambda e: e.tensor_copy(out=avb[:], in_=PF[1][0][:, :]), reads=["OB0"], writes=[an])
            return
        op("dve", lambda e: e.bn_stats(out=bst[:, 0:6], in_=avb[:]), reads=[an], writes=["bst"])
        op("dve", lambda e: e.bn_aggr(out=mv[:, 0:2], in_=bst[:, 0:6]), reads=["bst"], writes=["mv"])
        op("act", lambda e: e.activation(out=mv[:, 2:3], in_=mv[:, 1:2], func=AF.Ln, bias=epsb[:, 0:1], scale=1.0),
           reads=["mv", "epsb"], writes=["mv2"])
        op("act", lambda e: e.activation(out=mv[:, 3:4], in_=mv[:, 2:3], func=AF.Exp, scale=-0.5),
           reads=["mv2"], writes=["mv3"])
        op("dve", lambda e: e.tensor_scalar(out=avb[:], in0=avb[:], scalar1=mv[:, 0:1], scalar2=mv[:, 3:4],
                                            op0=ALU.subtract, op1=ALU.mult),
           reads=[an, "mv", "mv3"], writes=[an])
        op("dve", lambda e: e.tensor_tensor(out=avb[:], in0=avb[:], in1=lngb[:, 0:512], op=ALU.mult),
           reads=[an, "lngb"], writes=[an])
        op("dve", lambda e: e.tensor_tensor(out=vn[:], in0=avb[:], in1=lngb[:, 512:1024], op=ALU.add),
           reads=[an, "lngb"], writes=["vn"])
        for g in range(4):
            op("pe", lambda e, g=g: e.matmul(MM[2][:, 128 * g:128 * g + 128], lhsT=wsT[:, 128 * g:128 * g + 128],
                                             rhs=vn[:, 128 * g:128 * g + 128], start=True, stop=True),
               reads=["wsT", "vn"], writes=["MM2"])
        for g in range(4):
            op("dve", lambda e, g=g: e.scalar_tensor_tensor(
                out=cata[:, 128 * g:128 * g + 128], in0=MM[2][:, 128 * g:128 * g + 128], scalar=bsT[:, g:g + 1],
                in1=u_sb[:, 128 * g:128 * g + 128], op0=ALU.add, op1=ALU.mult),
               reads=["MM2", "bsT", un], writes=["cata"])
        for k in range(8):
            src = cata[:, 128 * k:128 * k + 128] if k < 4 else outb[:, i, 128 * (k - 4):128 * (k - 4) + 128]
            op("pe", lambda e, k=k, src=src: e.transpose(out=T1[:, 128 * k:128 * k + 128], in_=src, identity=ident[:]),
               reads=["cata", "outb:%d" % i, "ident"], writes=["T1"])
        op("dve", lambda e: e.tensor_copy(out=catT[:, 0:512], in_=T1[:, 0:512]), reads=["T1"], writes=["catTa"])
        op("dve", lambda e: e.tensor_copy(out=catT[:, 512:1024], in_=T1[:, 512:1024]), reads=["T1"], writes=["catTb"])
        for og in range(2):
            for k in range(8):
                op("pe", lambda e, k=k, og=og: e.matmul(
                    MM[og][:, :], lhsT=catT[:, 128 * k:128 * k + 128], rhs=wo[:, k, 512 * og:512 * og + 512],
                    start=(k == 0), stop=(k == 7)),
                   reads=["catTa" if k < 4 else "catTb", "wo_%d" % k], writes=["MM%d" % og])
        op("act", lambda e: e.activation(out=junk[:, 0:512], in_=MM[0][:, :], func=AF.Square, accum_out=ss[:, 2:3]),
           reads=["MM0"], writes=["ssa", "junk"])
        op("act", lambda e: e.activation(out=junk[:, 512:1024], in_=MM[1][:, :], func=AF.Square, accum_out=ss[:, 3:4]),
           reads=["MM1"], writes=["ssb", "junk"])
        op("dve", lambda e: e.tensor_tensor(out=ss[:, 1:2], in0=ss[:, 2:3], in1=ss[:, 3:4], op=ALU.add),
           reads=["ssa", "ssb"], writes=["ss1"])
        rms_rstd(None, 1)
        for og in range(2):
            op("dve", lambda e, og=og: e.scalar_tensor_tensor(
                out=tmpx[:, 512 * og:512 * og + 512], in0=MM[og][:, :], scalar=rstd[:, 1:2],
                in1=gpost[:, 512 * og:512 * og + 512], op0=ALU.mult, op1=ALU.mult),
               reads=["MM%d" % og, "rstd1", "gpost"], writes=["tmpx%d" % og])
        op("dve", lambda e: e.tensor_tensor(out=xs[:], in0=xs[:], in1=tmpx[:], op=ALU.add),
           reads=[xn, "tmpx0", "tmpx1"], writes=[xn])
        op("sp", lambda e, t0=t0: e.dma_start(out=y_d[t0:t0 + 128, :], in_=xs[:]), reads=[xn],
           writes=["y:%d" % i], dma="yst%d" % par)
        if debug:
            op("sp", lambda e, t0=t0: e.dma_start(out=dbg_x1[t0:t0 + 128, :], in_=xs[:]), reads=[xn], dma="dbg3")

    p1b_block(0, "F")
    for i in range(NB):
        la = sc.record(lambda: p1b_block(i, "K"))
        lb = sc.record(lambda: p1b_block(i + 1, "F")) if i + 1 < NB else []
        sc.interleave(la, lb)
        nper = (len(ffn_loads) + NB - 1) // NB
        for _ in range(nper):
            if ffn_loads:
                ffn_loads.pop(0)()
    while ffn_loads:
        ffn_loads.pop(0)()
    sc.regroup("wgu", ["wg_%d_%d" % (k, hf) for k in range(8) for hf in range(2)]
               + ["wu_%d_%d" % (k, hf) for k in range(8) for hf in range(2)])
    sc.barrier()

    check("P1b")
    p2 = None
    aset(P1B_BASE)
    wd = sb("wd", [128, NFF, D_MODEL], BF16, p2)
    aset(32 * 1024)
    gpost = sb("gpost2", [128, 1024], F32, p2)
    op("sp", lambda e: e.dma_start(out=gpost[:], in_=gpost_d[:, 1024:2048]), writes=["gpost"], dma="c3")
    aset(P1B_BASE + NFF * D_MODEL * 2)
    for c in range(NFF):
        op("pool", lambda e, c=c: e.dma_start(out=wd[:, c, :], in_=w_d_d[128 * c:128 * c + 128, :]),
           writes=["wd_%d" % c], dma="wd")
    sc.regroup("wd", ["wd_%d" % c for c in range(NFF)])
    actT = sb("actT", [128, NFF, 512], BF16, p2)
    aset(0)
    x1g = sb("x1g", [128, 4, 1024], F32, p2)
    h2T = sb("h2T", [128, 8, 512], BF16, p2)
    sg = [sb("sg%d" % j, [128, 512], F32, p2) for j in range(2)]
    outx = sb("outx", [128, 1024], F32, p2)
    assert apos[0] <= 32 * 1024
    GA = [MM[0], MM[1]]
    UB = [MM[2], S0]
    FB = [OB[0], OB[1]]
    GAn = ["MM0", "MM1"]
    UBn = ["MM2", "S0"]
    FBn = ["OB0", "OB1"]

    for g in range(NG):
        for b in range(4):
            i = 4 * g + b
            op("sp", lambda e, i=i, b=b: e.dma_start(out=x1g[:, b, :], in_=y_d[128 * i:128 * i + 128, :]),
               reads=["y:%d" % i], writes=["x1g%d" % b], dma="x1g%d" % b)
        for b in range(4):
            op("act", lambda e, b=b: e.activation(out=junk[:, 0:1024], in_=x1g[:, b, :], func=AF.Square,
                                                  accum_out=ss[:, 4 + b:5 + b]),
               reads=["x1g%d" % b], writes=["ss%d" % (4 + b), "junk"])
        for b in range(4):
            rms_rstd(None, 4 + b)
        for b in range(4):
            op("dve", lambda e, b=b: e.tensor_scalar(out=hb[:], in0=x1g[:, b, :], scalar1=rstd[:, 4 + b:5 + b],
                                                     scalar2=None, op0=ALU.mult),
               reads=["x1g%d" % b, "rstd%d" % (4 + b)], writes=["hb"])
            tb = b % 2
            for k in range(8):
                op("pe", lambda e, k=k, tb=tb: e.transpose(out=TB[tb][:, 128 * k:128 * k + 128],
                                                           in_=hb[:, 128 * k:128 * k + 128], identity=ident[:]),
                   reads=["hb", "ident"], writes=["T%d" % tb])
            for k in range(8):
                if True:
                    op("dve", lambda e, k=k, tb=tb, b=b: e.tensor_scalar(
                        out=h2T[:, k, 128 * b:128 * b + 128], in0=TB[tb][:, 128 * k:128 * k + 128],
                        scalar1=gcol[:, 8 + k:9 + k], scalar2=None, op0=ALU.mult),
                       reads=["T%d" % tb, "gcol"], writes=["h2T%d" % k])
                else:
                    op("act", lambda e, k=k, tb=tb, b=b: e.activation(
                        out=h2T[:, k, 128 * b:128 * b + 128], in_=TB[tb][:, 128 * k:128 * k + 128], func=AF.Identity,
                        scale=gcol[:, 8 + k:9 + k]),
                       reads=["T%d" % tb, "gcol"], writes=["h2T%d" % k])
        for c in range(NFF):
            j = c % 2
            for k in range(8):
                op("pe", lambda e, k=k, c=c, j=j: e.matmul(GA[j][:, :], lhsT=wg[:, k, 128 * c:128 * c + 128], rhs=h2T[:, k, :],
                                                           start=(k == 0), stop=(k == 7)),
                   reads=["wg_%d_%d" % (k, 0 if c < 11 else 1), "h2T%d" % k], writes=[GAn[j]])
            for k in range(8):
                op("pe", lambda e, k=k, c=c, j=j: e.matmul(UB[j][:, :], lhsT=wu[:, k, 128 * c:128 * c + 128], rhs=h2T[:, k, :],
                                                           start=(k == 0), stop=(k == 7)),
                   reads=["wu_%d_%d" % (k, 0 if c < 11 else 1), "h2T%d" % k], writes=[UBn[j]])
            op("act", lambda e, j=j: e.activation(out=sg[j][:], in_=GA[j][:, :], func=AF.Silu),
               reads=[GAn[j]], writes=["sg%d" % j])
            op("dve", lambda e, j=j, c=c: e.tensor_tensor(out=actT[:, c, :], in0=sg[j][:], in1=UB[j][:, :], op=ALU.mult),
               reads=["sg%d" % j, UBn[j]], writes=["actT%d" % c])
        for b in range(4):
            i = 4 * g + b
            for og in range(2):
                for c in range(NFF):
                    op("pe", lambda e, c=c, og=og, b=b: e.matmul(
                        FB[og][:, :], lhsT=actT[:, c, 128 * b:128 * b + 128], rhs=wd[:, c, 512 * og:512 * og + 512],
                        start=(c == 0), stop=(c == NFF - 1)),
                       reads=["actT%d" % c, "wd_%d" % c], writes=[FBn[og]])
            op("act", lambda e: e.activation(out=junk[:, 0:512], in_=FB[0][:, :], func=AF.Square, accum_out=ss[:, 2:3]),
               reads=["OB0"], writes=["ssa", "junk"])
            op("act", lambda e: e.activation(out=junk[:, 512:1024], in_=FB[1][:, :], func=AF.Square, accum_out=ss[:, 3:4]),
               reads=["OB1"], writes=["ssb", "junk"])
            op("dve", lambda e: e.tensor_tensor(out=ss[:, 1:2], in0=ss[:, 2:3], in1=ss[:, 3:4], op=ALU.add),
               reads=["ssa", "ssb"], writes=["ss1"])
            rms_rstd(None, 1)
            for og in range(2):
                op("dve", lambda e, og=og: e.scalar_tensor_tensor(
                    out=outx[:, 512 * og:512 * og + 512], in0=FB[og][:, :], scalar=rstd[:, 1:2],
                    in1=gpost[:, 512 * og:512 * og + 512], op0=ALU.mult, op1=ALU.mult),
                   reads=[FBn[og], "rstd1", "gpost"], writes=["outx%d" % og])
            op("dve", lambda e, b=b: e.tensor_tensor(out=outx[:], in0=outx[:], in1=x1g[:, b, :], op=ALU.add),
               reads=["outx0", "outx1", "x1g%d" % b], writes=["outx0", "outx1"])
            op("sp", lambda e, i=i: e.dma_start(out=y_d[128 * i:128 * i + 128, :], in_=outx[:]),
               reads=["outx0", "outx1"], writes=["y:%d" % i], dma="yst")
    return


def _t5_bucket(rel):
    rel = np.asarray(rel, np.int32)
    nb = 16
    ret = (rel > 0).astype(np.int32) * nb
    n = np.abs(rel)
    me = 8
    nf = np.maximum(n, 1).astype(np.float32)
    large = me + (np.log(nf / np.float32(me)) / np.float32(np.log(128 / me)) * np.float32(nb - me)).astype(np.int32)
    large = np.minimum(large, nb - 1)
    return ret + np.where(n < me, n, large)


def host_consts(inp):
    f32 = np.float32
    c = {}
    c["ident"] = np.eye(128, dtype=f32)
    pc = np.arange(128) // 64
    c["vis"] = np.where(pc[None, :] <= pc[:, None], 0.0, NEG).astype(f32)
    c["pw"] = np.broadcast_to((2.0 ** -np.arange(T_BIS + 1)).astype(f32), (128, T_BIS + 1)).copy()
    gcol = np.zeros((128, 16), f32)
    gcol[:, 0:8] = np.asarray(inp["g_pre_mix"], f32).reshape(8, 128).T
    gcol[:, 8:16] = np.asarray(inp["g_pre_ffn"], f32).reshape(8, 128).T
    c["gcol"] = gcol
    gpost = np.zeros((128, 2048), f32)
    gpost[:, 0:1024] = np.asarray(inp["g_post_mix"], f32).reshape(1, 1024)
    gpost[:, 1024:2048] = np.asarray(inp["g_post_ffn"], f32).reshape(1, 1024)
    c["gpost"] = gpost
    ln = np.zeros((128, 1024), f32)
    ln[:, 0:512] = np.asarray(inp["sgu_ln_g"], f32).reshape(1, 512)
    ln[:, 512:1024] = np.asarray(inp["sgu_ln_b"], f32).reshape(1, 512)
    c["lngb"] = ln
    rb = np.asarray(inp["rel_bias"], f32)
    p = np.arange(128)[:, None]
    j = np.arange(256)[None, :]
    bucket = _t5_bucket(j - 128 - p)
    bh = np.zeros((128, 4 * 256 + 4), f32)
    for h in range(4):
        bh[:, 256 * h:256 * h + 256] = rb[bucket, h]
        bh[:, 1024 + h] = rb[15, h]
    c["bh"] = bh
    sw = np.asarray(inp["sgu_w"], f32).reshape(4, 128, 128)
    c["wsT"] = np.ascontiguousarray(sw.transpose(2, 0, 1).reshape(128, 512))
    c["bsT"] = np.ascontiguousarray(np.asarray(inp["sgu_b"], f32).reshape(4, 128).T)
    return c


def make_in_maps(inputs, n_cores, S):
    c = host_consts(inputs)
    shared = dict(c)
    shared["w_in"] = np.ascontiguousarray(np.asarray(inputs["w_in"], np.float32).reshape(D_MODEL, 3144))
    shared["w_o"] = np.ascontiguousarray(np.asarray(inputs["w_o"], np.float32).reshape(D_MODEL, D_MODEL))
    shared["w_gate"] = np.ascontiguousarray(np.asarray(inputs["w_gate"], np.float32).reshape(D_MODEL, D_FF))
    shared["w_up"] = np.ascontiguousarray(np.asarray(inputs["w_up"], np.float32).reshape(D_MODEL, D_FF))
    shared["w_down"] = np.ascontiguousarray(np.asarray(inputs["w_down"], np.float32).reshape(D_FF, D_MODEL))
    x = np.asarray(inputs["x"], np.float32)
    maps = []
    for b in range(n_cores):
        m = dict(shared)
        m["x"] = np.ascontiguousarray(x[b, :S])
        maps.append(m)
    return maps


def kernel(**inputs):
    n = 8
    nc = build(NB=32, debug=False)
    in_maps = make_in_maps(inputs, n, SEQ)
    res = run_bass_kernel_spmd(nc, in_maps, core_ids=list(range(n)))
    out = np.stack([np.asarray(r["y"], np.float32) for r in res.results], axis=0)
    return out
```
